# Optimizing a Trainium2 kernel written in Bass

```python
import math
import jax, jax.numpy as jnp
from jax import lax
import numpy as np

D_MODEL = 4096
BATCH = 4
SEQ = 4096
DEPTH = 1

HEAD_DIM = 128
N_HEADS = D_MODEL // HEAD_DIM
HEADS_A = N_HEADS // 2
HEADS_B = N_HEADS - HEADS_A
WIDTH_A = HEADS_A * HEAD_DIM
WIDTH_B = HEADS_B * HEAD_DIM
MIX_WIDTH = WIDTH_A + WIDTH_B
DILATED_BRANCHES = ((128, 1), (512, 4), (2048, 16))
BLOCK_Q = 128
NUM_BUCKETS = 32
MAX_DISTANCE = 2048
FGATE_BIAS_MEAN = 2.0
PEER_HEADS = 8
N_KEYS = 128
N_EXPERTS = N_KEYS * N_KEYS
PEER_TOPK = 16
PEER_QUERY_DIM = 256
PEER_HALF = PEER_QUERY_DIM // 2
PEER_CHUNK = 64
N_MOD = 6
NORM_EPS = 1e-6
NEG_INF = -1e30
IN_COLS = 3 * WIDTH_A + 3 * WIDTH_B + HEADS_B

kernel_name = 'hybrid_dilated_fox_peer_layer'


def rmsnorm(x, g):
    x32 = x.astype(jnp.float32)
    y = x32 * lax.rsqrt(jnp.mean(x32 * x32, axis=-1, keepdims=True) + NORM_EPS)
    return (y * g.astype(jnp.float32)).astype(x.dtype)


def t5_bucket(dist):
    max_exact = NUM_BUCKETS // 2
    d32 = jnp.maximum(dist, 1).astype(jnp.float32)
    large = max_exact + (jnp.log(d32 / max_exact) / math.log(MAX_DISTANCE / max_exact)
                         * (NUM_BUCKETS - max_exact)).astype(jnp.int32)
    large = jnp.minimum(large, NUM_BUCKETS - 1)
    return jnp.where(dist < max_exact, dist, large)


def dilated_branch(q, k, v, rel_bias, window, dilation):
    B, H, S, Dh = q.shape
    L = S // dilation
    nw = window // dilation
    bq = min(BLOCK_Q, L)
    nblk = L // bq

    def to_sub(t):
        return t.reshape(B, H, L, dilation, Dh).transpose(0, 1, 3, 2, 4)

    qs = to_sub(q).reshape(B, H, dilation, nblk, bq, Dh)
    pad = ((0, 0), (0, 0), (0, 0), (nw, 0), (0, 0))
    ks = jnp.pad(to_sub(k), pad)
    vs = jnp.pad(to_sub(v), pad)
    idx = jnp.arange(nblk)[:, None] * bq + jnp.arange(bq + nw)[None, :]
    kb = ks[:, :, :, idx]
    vb = vs[:, :, :, idx].astype(jnp.float32)
    logits = jnp.einsum('bhrnid,bhrnjd->bhrnij', qs, kb).astype(jnp.float32) * (HEAD_DIM ** -0.5)
    rel = jnp.arange(bq)[:, None] + nw - jnp.arange(bq + nw)[None, :]
    in_win = (rel >= 0) & (rel <= nw)
    bucket = t5_bucket(jnp.clip(rel, 0, nw) * dilation)
    bias = jnp.transpose(rel_bias[bucket], (2, 0, 1)).astype(jnp.float32)
    key_ok = (idx - nw) >= 0
    mask = in_win[None, :, :] & key_ok[:, None, :]
    logits = jnp.where(mask, logits + bias[:, None, None], NEG_INF)
    m = jnp.max(logits, axis=-1, keepdims=True)
    p = jnp.exp(logits - m)
    denom = jnp.sum(p, axis=-1, keepdims=True)
    num = jnp.einsum('bhrnij,bhrnjd->bhrnid', p, vb)

    def from_sub(t):
        return t.reshape(B, H, dilation, L, -1).transpose(0, 1, 3, 2, 4).reshape(B, H, S, -1)

    return from_sub(num), from_sub(m), from_sub(denom)


def dilated_attention(q, k, v, rel_bias):
    outs = [dilated_branch(q, k, v, rel_bias, w, d) for (w, d) in DILATED_BRANCHES]
    m_all = outs[0][1]
    for o in outs[1:]:
        m_all = jnp.maximum(m_all, o[1])
    num = outs[0][0] * jnp.exp(outs[0][1] - m_all)
    den = outs[0][2] * jnp.exp(outs[0][1] - m_all)
    for o in outs[1:]:
        w = jnp.exp(o[1] - m_all)
        num = num + o[0] * w
        den = den + o[2] * w
    return num / den


def forgetting_attention(q, k, v, log_f):
    B, H, S, Dh = q.shape
    nq = S // BLOCK_Q
    F = jnp.cumsum(log_f, axis=-1)
    k32 = k.astype(jnp.float32)
    v32 = v.astype(jnp.float32)
    qb = q.astype(jnp.float32).reshape(B, H, nq, BLOCK_Q, Dh).transpose(2, 0, 1, 3, 4)
    Fq = F.reshape(B, H, nq, BLOCK_Q).transpose(2, 0, 1, 3)
    starts = jnp.arange(nq, dtype=jnp.int32) * BLOCK_Q
    kpos = jnp.arange(S, dtype=jnp.int32)
    scale = HEAD_DIM ** -0.5

    def block(args):
        qi, Fi, s0 = args
        logits = jnp.einsum('bhid,bhjd->bhij', qi, k32) * scale + Fi[..., :, None] - F[..., None, :]
        causal = kpos[None, :] <= (s0 + jnp.arange(BLOCK_Q, dtype=jnp.int32))[:, None]
        p = jax.nn.softmax(jnp.where(causal, logits, NEG_INF), axis=-1)
        return jnp.einsum('bhij,bhjd->bhid', p, v32)

    out = lax.map(block, (qb, Fq, starts))
    return out.transpose(1, 2, 0, 3, 4).reshape(B, H, S, Dh)


def peer(h, w_pq, sk1, sk2, expert_u, expert_v):
    B, S, D = h.shape
    T = B * S
    ht = h.reshape(T, D)
    q = jnp.dot(ht, w_pq).reshape(T, PEER_HEADS, 2, PEER_HALF)
    s1 = jnp.einsum('thd,kd->thk', q[:, :, 0], sk1).astype(jnp.float32)
    s2 = jnp.einsum('thd,kd->thk', q[:, :, 1], sk2).astype(jnp.float32)
    v1, i1 = lax.top_k(s1, PEER_TOPK)
    v2, i2 = lax.top_k(s2, PEER_TOPK)
    cand = (v1[..., :, None] + v2[..., None, :]).reshape(T, PEER_HEADS, PEER_TOPK * PEER_TOPK)
    vs, ci = lax.top_k(cand, PEER_TOPK)
    e1 = jnp.take_along_axis(i1, ci // PEER_TOPK, axis=-1)
    e2 = jnp.take_along_axis(i2, ci % PEER_TOPK, axis=-1)
    eid = (e1 * N_KEYS + e2).reshape(T, PEER_HEADS * PEER_TOPK)
    gate = jax.nn.softmax(vs, axis=-1).reshape(T, PEER_HEADS * PEER_TOPK)
    nch = T // PEER_CHUNK

    def chunk(args):
        hc, ec, gc = args
        a = jnp.einsum('ced,cd->ce', expert_u[ec], hc).astype(jnp.float32)
        act = jax.nn.gelu(a, approximate=False) * gc
        return jnp.einsum('ce,ced->cd', act, expert_v[ec].astype(jnp.float32))

    y = lax.map(chunk, (ht.reshape(nch, PEER_CHUNK, D),
                        eid.reshape(nch, PEER_CHUNK, -1),
                        gate.reshape(nch, PEER_CHUNK, -1)))
    return y.reshape(B, S, D).astype(h.dtype)


def hybrid_layer(x, c, w_ada, b_ada, norm1_g, w_in, b_f, q_norm_a, k_norm_a, q_norm_b, k_norm_b,
                 rel_bias, w_o, norm2_g, w_pq, sub_keys_1, sub_keys_2, expert_u, expert_v):
    B, S, D = x.shape
    mod = jnp.dot(jax.nn.silu(c), w_ada) + b_ada
    sh1, sc1, g1, sh2, sc2, g2 = jnp.split(mod[:, None, :], N_MOD, axis=-1)

    h = rmsnorm(x, norm1_g) * (1.0 + sc1) + sh1
    proj = jnp.dot(h, w_in)
    bounds = [WIDTH_A, 2 * WIDTH_A, 3 * WIDTH_A, 3 * WIDTH_A + WIDTH_B,
              3 * WIDTH_A + 2 * WIDTH_B, 3 * WIDTH_A + 3 * WIDTH_B]
    qa, ka, va, qb, kb, vb, fz = jnp.split(proj, bounds, axis=-1)

    def heads(t, n):
        return t.reshape(B, S, n, HEAD_DIM).transpose(0, 2, 1, 3)

    qa = rmsnorm(heads(qa, HEADS_A), q_norm_a)
    ka = rmsnorm(heads(ka, HEADS_A), k_norm_a)
    va = heads(va, HEADS_A)
    qb = rmsnorm(heads(qb, HEADS_B), q_norm_b)
    kb = rmsnorm(heads(kb, HEADS_B), k_norm_b)
    vb = heads(vb, HEADS_B)
    log_f = jax.nn.log_sigmoid((fz + b_f).astype(jnp.float32)).transpose(0, 2, 1)

    out_a = dilated_attention(qa, ka, va, rel_bias)
    out_b = forgetting_attention(qb, kb, vb, log_f)
    mix = jnp.concatenate([out_a, out_b], axis=1).transpose(0, 2, 1, 3).reshape(B, S, MIX_WIDTH)
    x = x + g1 * jnp.dot(mix.astype(x.dtype), w_o)

    h2 = rmsnorm(x, norm2_g) * (1.0 + sc2) + sh2
    x = x + g2 * peer(h2, w_pq, sub_keys_1, sub_keys_2, expert_u, expert_v)
    return x


def setup_inputs(seed: int = 0) -> dict:
    key = jax.random.key(seed)
    ks = jax.random.split(key, 20)
    f32 = jnp.float32
    dsc = D_MODEL ** -0.5
    nrm = lambda k, shape: jax.random.normal(k, shape, f32)
    return {
        'x': nrm(ks[0], (BATCH, SEQ, D_MODEL)),
        'c': nrm(ks[1], (BATCH, D_MODEL)),
        'w_ada': nrm(ks[2], (DEPTH, D_MODEL, N_MOD * D_MODEL)) * (0.5 * dsc),
        'b_ada': nrm(ks[3], (DEPTH, N_MOD * D_MODEL)) * 0.02,
        'norm1_g': 1.0 + 0.02 * nrm(ks[4], (DEPTH, D_MODEL)),
        'w_in': nrm(ks[5], (DEPTH, D_MODEL, IN_COLS)) * dsc,
        'b_f': FGATE_BIAS_MEAN + 0.1 * nrm(ks[6], (DEPTH, HEADS_B)),
        'q_norm_a': 1.0 + 0.02 * nrm(ks[7], (DEPTH, HEAD_DIM)),
        'k_norm_a': 1.0 + 0.02 * nrm(ks[8], (DEPTH, HEAD_DIM)),
        'q_norm_b': 1.0 + 0.02 * nrm(ks[9], (DEPTH, HEAD_DIM)),
        'k_norm_b': 1.0 + 0.02 * nrm(ks[10], (DEPTH, HEAD_DIM)),
        'rel_bias': 0.5 * nrm(ks[11], (NUM_BUCKETS, HEADS_A)),
        'w_o': nrm(ks[12], (DEPTH, MIX_WIDTH, D_MODEL)) * (MIX_WIDTH ** -0.5),
        'norm2_g': 1.0 + 0.02 * nrm(ks[13], (DEPTH, D_MODEL)),
        'w_pq': nrm(ks[14], (DEPTH, D_MODEL, PEER_HEADS * PEER_QUERY_DIM)) * dsc,
        'sub_keys_1': nrm(ks[15], (DEPTH, N_KEYS, PEER_HALF)) * (PEER_HALF ** -0.5),
        'sub_keys_2': nrm(ks[16], (DEPTH, N_KEYS, PEER_HALF)) * (PEER_HALF ** -0.5),
        'expert_u': nrm(ks[17], (DEPTH, N_EXPERTS, D_MODEL)) * dsc,
        'expert_v': nrm(ks[18], (DEPTH, N_EXPERTS, D_MODEL)) * 0.5,
    }


def reference(x, c, w_ada, b_ada, norm1_g, w_in, b_f, q_norm_a, k_norm_a, q_norm_b, k_norm_b,
              rel_bias, w_o, norm2_g, w_pq, sub_keys_1, sub_keys_2, expert_u, expert_v):
    for l in range(DEPTH):
        x = hybrid_layer(x, c, w_ada[l], b_ada[l], norm1_g[l], w_in[l], b_f[l],
                         q_norm_a[l], k_norm_a[l], q_norm_b[l], k_norm_b[l], rel_bias,
                         w_o[l], norm2_g[l], w_pq[l], sub_keys_1[l], sub_keys_2[l],
                         expert_u[l], expert_v[l])
    return x
```

```python
import math
from contextlib import ExitStack
import numpy as np
import concourse.bass as bass
import concourse.mybir as mybir
from concourse.bass_utils import run_bass_kernel_spmd

F32 = mybir.dt.float32
BF16 = mybir.dt.bfloat16
I32 = mybir.dt.int32
AF = mybir.ActivationFunctionType
ALU = mybir.AluOpType

ENGS = ("tensor", "vector", "scalar", "gpsimd", "sync")
NEG = -1e30
EPS = 1e-6


class Tile:
    def __init__(self, ctx, h, name):
        self.ctx = ctx
        self.h = h
        self.name = name
        self.w = []
        self.r = []
        self.dsem = None
        self.dram = False

    def __getitem__(self, idx):
        return V(self, self.h[idx])

    def v(self, ap):
        return V(self, ap)

    def dap(self, off, dims):
        return V(self, bass.AP(self.h, off, [list(d) for d in dims]))


class V:
    def __init__(self, tile, ap):
        self.t = tile
        self.ap = ap

    def __getitem__(self, idx):
        return V(self.t, self.ap[idx])

    def re(self, s, **kw):
        return V(self.t, self.ap.rearrange(s, **kw))

    def bc(self, shape):
        return V(self.t, self.ap.broadcast_to(list(shape)))

    def unsq(self, ax):
        return V(self.t, self.ap.unsqueeze(ax))

    def bitcast(self, dt):
        return V(self.t, self.ap.bitcast(dt))


def _ap(x):
    return x.ap if isinstance(x, V) else x


class Ctx:
    def __init__(self, nc):
        self.nc = nc
        self.sems = {}
        self.semval = {}
        self.ops = {e: [] for e in ENGS}
        self.waited = {e: {} for e in ENGS}
        self.ninst = {e: 0 for e in ENGS}
        self.dyn = {}
        for e in ENGS:
            self._newsem("E_" + e)

    def _newsem(self, key):
        self.sems[key] = self.nc.alloc_semaphore(name=key)
        self.semval[key] = 0
        return key

    def tile(self, h, name):
        return Tile(self, h, name)

    def _deps(self, eng, reads, writes, is_dma=False):
        ev = []
        for t in reads:
            if t is not None:
                ev.extend(t.w)
        for t in writes:
            if t is None:
                continue
            for (k, v, e) in t.w:
                if is_dma and e == "dma":
                    continue
                if (not is_dma) and e == eng and eng == "tensor":
                    continue
                ev.append((k, v, e))
            for (k, v, e) in t.r:
                if (not is_dma) and e == eng and eng == "tensor":
                    continue
                ev.append((k, v, e))
        need = {}
        for (k, v, e) in ev:
            if v > need.get(k, 0):
                need[k] = v
        out = []
        w = self.waited[eng]
        for k, v in need.items():
            if w.get(k, 0) >= v:
                continue
            w[k] = v
            out.append((k, v))
        return out

    def _commit(self, reads, writes, event):
        for t in writes:
            if t is None:
                continue
            if event[2] == "dma":
                t.w = [x for x in t.w if x[2] == "dma" and x[0] != event[0]] + [event]
            else:
                t.w = [event]
            t.r = []
        for t in reads:
            if t is None or t in writes:
                continue
            t.r = [x for x in t.r if x[0] != event[0]] + [event]

    def op(self, eng, fn, reads=(), writes=()):
        rt = [x.t if isinstance(x, V) else x for x in reads]
        wt = [x.t if isinstance(x, V) else x for x in writes]
        waits = self._deps(eng, rt, wt)
        key = "E_" + eng
        self.semval[key] += 1
        val = self.semval[key]
        sems = self.sems

        def emit(e, waits=waits, fn=fn, key=key):
            for (k, v) in waits:
                e.wait_ge(sems[k], v)
            fn(e).then_inc(sems[key], 1)

        self.ops[eng].append(emit)
        self.ninst[eng] += 1
        self._commit(rt, wt, (key, val, eng))

    def dma(self, eng, out, in_, in_fn=None, **kw):
        ot = out.t if isinstance(out, V) else None
        it = in_.t if isinstance(in_, V) else None
        cands = [t for t in (ot, it) if t is not None]
        sbs = [t for t in cands if not t.dram]
        owner = sbs[0] if sbs else cands[0]
        if owner.dsem is None:
            owner.dsem = self._newsem("D%d_%s" % (len(self.sems), owner.name))
        key = owner.dsem
        waits = self._deps(eng, [it], [ot], is_dma=True)
        self.semval[key] += 16
        val = self.semval[key]
        sems = self.sems
        oap, iap = _ap(out), _ap(in_)

        def emit(e, waits=waits):
            for (k, v) in waits:
                e.wait_ge(sems[k], v)
            src = in_fn() if in_fn is not None else iap
            e.dma_start(out=oap, in_=src, **kw).then_inc(sems[key], 16)

        self.ops[eng].append(emit)
        self.ninst[eng] += 1
        self._commit([it], [ot], (key, val, "dma"))

    def collective(self, kind, op, groups, in_, out):
        eng = "gpsimd"
        it, ot = in_.t, out.t
        if ot.dsem is None:
            ot.dsem = self._newsem("D%d_%s" % (len(self.sems), ot.name))
        key = ot.dsem
        waits = self._deps(eng, [it], [ot], is_dma=True)
        self.semval[key] += 1
        val = self.semval[key]
        sems = self.sems
        oap, iap = _ap(out), _ap(in_)

        def emit(e, waits=waits):
            for (k, v) in waits:
                e.wait_ge(sems[k], v)
            e.collective_compute(kind, op, replica_groups=groups, ins=[iap], outs=[oap]).then_inc(sems[key], 1)

        self.ops[eng].append(emit)
        self._commit([it], [ot], (key, val, "dma"))

    def raw(self, eng, fn):
        self.ops[eng].append(fn)

    def wait_all(self, eng):
        waits = []
        w = self.waited[eng]
        for k, v in self.semval.items():
            if v > 0 and w.get(k, 0) < v:
                w[k] = v
                waits.append((k, v))
        sems = self.sems

        def emit(e, waits=waits):
            for (k, v) in waits:
                e.wait_ge(sems[k], v)

        self.ops[eng].append(emit)

    def flush(self, block):
        for name in ENGS:
            lst = self.ops[name]
            if not lst:
                continue

            def body(e, lst=lst):
                for f in lst:
                    f(e)

            getattr(block, name)(body)
            self.ops[name] = []


class Ring:
    def __init__(self, tiles):
        self.tiles = tiles
        self.i = 0

    def next(self):
        t = self.tiles[self.i % len(self.tiles)]
        self.i += 1
        return t


class K:
    def __init__(self, ctx):
        self.c = ctx

    def mm(self, out, lhsT, rhs, start=True, stop=True):
        self.c.op("tensor", lambda e, o=out.ap, l=lhsT.ap, r=rhs.ap, s=start, p=stop:
                  e.matmul(o, lhsT=l, rhs=r, start=s, stop=p), [lhsT, rhs], [out])

    def tr(self, out, in_, ident):
        self.c.op("tensor", lambda e, o=out.ap, i=in_.ap, d=ident.ap: e.transpose(o, i, d), [in_, ident], [out])

    def act(self, out, in_, func, bias=None, scale=None, accum=None):
        rd = [in_]
        kw = {}
        if bias is not None:
            if isinstance(bias, V):
                rd.append(bias)
                kw["bias"] = bias.ap
            else:
                kw["bias"] = bias
        if scale is not None:
            if isinstance(scale, V):
                rd.append(scale)
                kw["scale"] = scale.ap
            else:
                kw["scale"] = scale
        wr = [out]
        if accum is not None:
            kw["accum_out"] = accum.ap
            wr.append(accum)
        self.c.op("scalar", lambda e, o=out.ap, i=in_.ap, f=func, kw=kw: e.activation(out=o, in_=i, func=f, **kw), rd, wr)

    def ts(self, out, in0, s1, op0, s2=None, op1=None, eng="vector"):
        rd = [in0]
        a1 = s1.ap if isinstance(s1, V) else s1
        a2 = s2.ap if isinstance(s2, V) else s2
        if isinstance(s1, V):
            rd.append(s1)
        if isinstance(s2, V):
            rd.append(s2)
        kw = {}
        if op1 is not None:
            kw["op1"] = op1
        self.c.op(eng, lambda e, o=out.ap, i=in0.ap, a1=a1, a2=a2, op0=op0, kw=kw:
                  e.tensor_scalar(out=o, in0=i, scalar1=a1, scalar2=a2, op0=op0, **kw), rd, [out])

    def tt(self, out, in0, in1, op, eng="vector"):
        self.c.op(eng, lambda e, o=out.ap, a=in0.ap, b=in1.ap, op=op: e.tensor_tensor(out=o, in0=a, in1=b, op=op),
                  [in0, in1], [out])

    def stt(self, out, in0, s, in1, op0, op1):
        rd = [in0, in1]
        a = s.ap if isinstance(s, V) else s
        if isinstance(s, V):
            rd.append(s)
        self.c.op("vector", lambda e, o=out.ap, i0=in0.ap, a=a, i1=in1.ap, op0=op0, op1=op1:
                  e.scalar_tensor_tensor(out=o, in0=i0, scalar=a, in1=i1, op0=op0, op1=op1), rd, [out])

    def cp(self, out, in_, eng="vector"):
        if eng == "scalar":
            self.c.op("scalar", lambda e, o=out.ap, i=in_.ap: e.copy(out=o, in_=i), [in_], [out])
        else:
            self.c.op(eng, lambda e, o=out.ap, i=in_.ap: e.tensor_copy(out=o, in_=i), [in_], [out])

    def max8(self, out, in_):
        self.c.op("vector", lambda e, o=out.ap, i=in_.ap: e.max(out=o, in_=i), [in_], [out])

    def mrep(self, out, rep, vals):
        self.c.op("vector", lambda e, o=out.ap, r=rep.ap, v=vals.ap:
                  e.match_replace(out=o, in_to_replace=r, in_values=v, imm_value=NEG), [rep, vals], [out])

    def scan_add(self, out, ones, data):
        self.c.op("vector", lambda e, o=out.ap, d0=ones.ap, d1=data.ap:
                  e.tensor_tensor_scan(out=o, data0=d0, data1=d1, initial=0.0, op0=ALU.mult, op1=ALU.add),
                  [ones, data], [out])

    def memset(self, out, val, eng="vector"):
        self.c.op(eng, lambda e, o=out.ap, v=val: e.memset(o, v), [], [out])

    def rmax_abs(self, out, in_):
        self.c.op("vector", lambda e, o=out.ap, i=in_.ap:
                  e.tensor_reduce(out=o, in_=i, axis=mybir.AxisListType.X, op=ALU.max, apply_absolute_value=True),
                  [in_], [out])


def _fact(n, cap=2048):
    for c in range(cap, 0, -1):
        if n % c == 0:
            return c
    return 1


def make_cfg(D, S):
    c = dict(D=D, S=S, B=4, KC=D // 128, NH=D // 128)
    c["HA"] = c["HB"] = c["NH"] // 2
    c["nA"] = c["HA"]
    c["nB"] = c["HB"]
    c["KC8"] = D // 8 // 128
    c["NE"] = 16384
    c["PH"] = 8
    c["BR"] = ((128, 1), (512, 4), (2048, 16))
    nl = c["nA"] + c["nB"]
    CH = 128 * 2048

    def pad(n, nr):
        q = nr * CH
        return (n + q - 1) // q * q
    c["CH"] = CH
    c["win_n"] = pad(nl * 128 * c["KC"] * 384 + 128 * c["KC"] * c["nB"], 1)
    c["wo_n"] = pad(D * D, 1)
    c["wpq_n"] = pad(D * 2048, 1)
    c["uT_n"] = pad(c["NE"] * D, 1)
    c["ev_n"] = pad(c["NE"] * D, 1)
    return c


def _t5_bucket(dist):
    max_exact = 16
    d32 = np.maximum(dist, 1).astype(np.float32)
    large = max_exact + (np.log(d32 / max_exact) / math.log(2048 / max_exact) * (32 - max_exact)).astype(np.int32)
    large = np.minimum(large, 31)
    return np.where(dist < max_exact, dist, large)


def host_prep(inp, cfg):
    D, S, KC, KC8 = cfg["D"], cfg["S"], cfg["KC"], cfg["KC8"]
    HA, HB, nA, nB = cfg["HA"], cfg["HB"], cfg["nA"], cfg["nB"]
    WA = HA * 128
    WB = HB * 128
    f32 = np.float32
    x = np.asarray(inp["x"], f32)
    c = np.asarray(inp["c"], f32)
    w_ada = np.asarray(inp["w_ada"], f32)[0]
    b_ada = np.asarray(inp["b_ada"], f32)[0]
    w_in = np.asarray(inp["w_in"], f32)[0]
    w_o = np.asarray(inp["w_o"], f32)[0]
    w_pq = np.asarray(inp["w_pq"], f32)[0]
    eu = np.asarray(inp["expert_u"], f32)[0]
    ev = np.asarray(inp["expert_v"], f32)[0]
    rel_bias = np.asarray(inp["rel_bias"], f32)
    b_f = np.asarray(inp["b_f"], f32)[0]

    def fm(vec, n):
        return np.ascontiguousarray(vec.reshape(n, 128).T)

    common = {}
    common["badaT"] = fm(b_ada, 6 * KC)
    common["n1g"] = fm(np.asarray(inp["norm1_g"], f32)[0], KC)
    common["n2g"] = fm(np.asarray(inp["norm2_g"], f32)[0], KC)
    common["qkn"] = np.ascontiguousarray(np.stack([np.asarray(inp[k], f32)[0] for k in
                                                   ("q_norm_a", "k_norm_a", "q_norm_b", "k_norm_b")], axis=1))
    common["relb"] = np.ascontiguousarray(rel_bias.reshape(1, -1))
    sk = np.stack([np.asarray(inp["sub_keys_1"], f32)[0], np.asarray(inp["sub_keys_2"], f32)[0]], axis=0)
    common["skT"] = np.ascontiguousarray(sk.transpose(2, 0, 1))
    common["ident"] = np.eye(128, dtype=f32)
    ii = np.arange(128)
    prevm = np.where(ii[:, None] >= ii[None, :], 0.0, NEG).astype(f32)
    diagm = np.where(ii[:, None] <= ii[None, :], 0.0, NEG).astype(f32)
    alln = np.full((128, 128), NEG, f32)
    common["maskn"] = np.ascontiguousarray(np.concatenate([prevm, diagm, prevm, diagm], axis=1))
    common["maskf"] = np.ascontiguousarray(np.concatenate([alln, diagm, prevm, diagm], axis=1))
    common["maskd"] = diagm

    def head_slab(colq, colk, colv):
        cols = np.concatenate([np.arange(colq, colq + 128), np.arange(colk, colk + 128), np.arange(colv, colv + 128)])
        w = w_in[:, cols]
        return w.reshape(KC, 128, 384).transpose(1, 0, 2)

    win_g = []
    for g in range(1):
        parts = []
        for i in range(nA):
            h = g * nA + i
            parts.append(head_slab(h * 128, WA + h * 128, 2 * WA + h * 128).ravel())
        for i in range(nB):
            h = g * nB + i
            parts.append(head_slab(3 * WA + h * 128, 3 * WA + WB + h * 128, 3 * WA + 2 * WB + h * 128).ravel())
        fzc = w_in[:, 3 * WA + 3 * WB + g * nB: 3 * WA + 3 * WB + (g + 1) * nB]
        parts.append(fzc.reshape(KC, 128, nB).transpose(1, 0, 2).ravel())
        win_g.append(np.concatenate(parts))
    wo_p = w_o
    NG = D // 256
    wo_l = wo_p.reshape(KC, 128, NG, 256).transpose(2, 1, 0, 3).ravel()
    wpq_l = w_pq.reshape(KC, 128, 16, 128).transpose(2, 1, 0, 3).ravel()
    uT_l = eu.reshape(128, 128, KC, 128).transpose(0, 3, 2, 1).ravel()
    ev_l = ev.ravel()

    def bias_tiles(h):
        out = np.zeros((3, 128, 256), f32)
        for bi, (w, d) in enumerate(cfg["BR"]):
            nw = w // d
            relp = ii[None, :] + 128 - ii[:, None]
            reld = ii[None, :] - ii[:, None]
            for o, rel in ((0, relp), (128, reld)):
                bk = _t5_bucket(np.clip(rel, 0, nw) * d)
                out[bi, :, o:o + 128] = rel_bias[bk, h]
        return out

    CH = cfg["CH"]

    def ishard(arr, tot, nr, rk):
        a = np.zeros(tot, f32)
        a[:arr.size] = arr
        return np.ascontiguousarray(a.reshape(-1, nr, CH)[:, rk, :].reshape(-1, 2048))

    cT_full = np.ascontiguousarray(c.reshape(4, KC, 128).transpose(2, 1, 0))
    wada_full = np.ascontiguousarray(w_ada.reshape(KC, 128, 6 * KC, 128).transpose(2, 1, 0, 3)).reshape(6 * KC, 128, KC * 128)
    wfull = {"win0": ishard(win_g[0], cfg["win_n"], 1, 0)}
    wfull["win1"] = wfull["win0"]
    bias_all = np.ascontiguousarray(np.stack([bias_tiles(i) for i in range(nA)], axis=0))
    for nm, arr, tot in (("wo", wo_l, cfg["wo_n"]), ("wpq", wpq_l, cfg["wpq_n"]),
                         ("uT", uT_l, cfg["uT_n"]), ("ev", ev_l, cfg["ev_n"])):
        wfull[nm] = ishard(arr, tot, 1, 0)
    maps = []
    for r in range(8):
        b, g = r // 2, r % 2
        m = dict(common)
        m["xf"] = np.ascontiguousarray(x[b])
        m["xh"] = np.ascontiguousarray(x[b, g * (S // 2):(g + 1) * (S // 2)])
        m["cT"] = cT_full
        m["wada"] = wada_full
        oh = np.zeros((128, 4), f32)
        oh[:, b] = 1.0
        m["oh"] = oh
        m["offs"] = np.array([[g * 128 * (S // 2), g * (S // 2) * D]], np.int32)
        m["win"] = wfull["win%d" % g]
        for nm in ("wo", "wpq", "uT", "ev"):
            m[nm] = wfull[nm]
        m["biasT"] = bias_all
        m["bf"] = np.ascontiguousarray(b_f.reshape(nB, 1))
        maps.append(m)
    return maps


def build(cfg, dbg=False, stop_after=None):
    D, S, KC, KC8 = cfg["D"], cfg["S"], cfg["KC"], cfg["KC8"]
    nA, nB = cfg["nA"], cfg["nB"]
    NL = nA + nB
    SH = S // 2
    NE = cfg["NE"]
    nc = bass.Bass("TRN2", target_bir_lowering=False)
    ctx = Ctx(nc)
    k = K(ctx)
    _cnt = [0]

    def uniq(name):
        _cnt[0] += 1
        return "s%d_%s" % (_cnt[0], name)

    def din(name, shape, dt=F32):
        return nc.dram_tensor(name, list(shape), dt, kind="ExternalInput")

    def dint(name, shape, dt):
        t = ctx.tile(nc.dram_tensor(name, list(shape), dt, kind="Internal"), name)
        t.dram = True
        return t

    xf_d = din("xf", [S, D])
    xh_d = din("xh", [SH, D])
    cT_d = din("cT", [128, KC, 4])
    wada_d = din("wada", [6 * KC, 128, KC * 128])
    badaT_d = din("badaT", [128, 6 * KC])
    n1g_d = din("n1g", [128, KC])
    n2g_d = din("n2g", [128, KC])
    qkn_d = din("qkn", [128, 4])
    relb_d = din("relb", [1, 32 * cfg["HA"]])
    skT_d = din("skT", [128, 2, 128])
    ident_d = din("ident", [128, 128])
    maskn_d = din("maskn", [128, 512])
    maskf_d = din("maskf", [128, 512])
    maskd_d = din("maskd", [128, 128])
    oh_d = din("oh", [128, 4])
    offs_d = din("offs", [1, 2], I32)
    biasT_d = din("biasT", [nA, 3, 128, 256])
    bf_d = din("bf", [nB, 1])
    wsh = {}
    for nm, tot, nr in (("win", cfg["win_n"], 1), ("wo", cfg["wo_n"], 1), ("wpq", cfg["wpq_n"], 1),
                        ("uT", cfg["uT_n"], 1), ("ev", cfg["ev_n"], 1)):
        n = tot // nr
        cc = 2048
        wsh[nm] = (din(nm, [n // cc, cc]), n // cc, cc, nr)
    out_d = nc.dram_tensor("out", [SH, D], F32, kind="ExternalOutput")
    dbg_out = {}

    def dbgout(name, shape, dt=F32):
        dbg_out[name] = nc.dram_tensor("dbg_" + name, list(shape), dt, kind="ExternalOutput")
        return dbg_out[name]

    wsh_b = {}
    wg = {}
    for nm, (h, n, cc, nr) in wsh.items():
        wg[nm] = dint(nm + "_g", [n, cc], BF16)
    gvec_d = dint("gvec", [2 * KC, 128], F32)
    KQ = KC // 4
    NPC = (SH // 256) * 4
    hTg_d = dint("hTg", [NPC * 2 * 128, KQ * 256], BF16)
    mixg_d = dint("mixg", [NL * 2 * 128, SH], BF16)
    x1_d = dint("x1", [SH, D], F32)
    h2T_d = dint("h2T", [128, KC * SH], BF16)

    G8 = [list(range(8))]
    GP = [[0, 1], [2, 3], [4, 5], [6, 7]]
    GQ = [[0, 2, 4, 6], [1, 3, 5, 7]]

    def cast_gather(nm, groups):
        h, n, cc, nr = wsh[nm]
        step = 2048
        for r0 in range(0, n, step):
            r1 = min(n, r0 + step)
            ctx.dma("gpsimd", wg[nm].v(wg[nm].h.ap()[r0:r1, :]), h.ap()[r0:r1, :])
        return lambda: None

    with ExitStack() as pes:
        def psb(name, shape, dt=F32):
            return ctx.tile(pes.enter_context(nc.sbuf_tensor(uniq(name), list(shape), dt)), name)

        ident = psb("ident", [128, 128])
        identb = psb("identb", [128, 128], BF16)
        ones_f = psb("ones_f", [128, 128])
        ones_b = psb("ones_b", [128, 128], BF16)
        msel = psb("msel", [128, 6 * KC])
        gm1 = psb("gm1", [128, KC])
        gm2 = psb("gm2", [128, KC])
        qkn = psb("qkn", [128, 4])
        qkg = psb("qkg", [128, 4])
        nbound = psb("nbound", [128, 2])
        offs_sb = psb("offs_sb", [1, 2], I32)
        reg_tok = pes.enter_context(nc.sync.register("r_tok"))
        reg_x = pes.enter_context(nc.sync.register("r_x"))

        def resnap(e):
            ctx.dyn["tok"] = e.snap(reg_tok)
            ctx.dyn["x"] = e.snap(reg_x)

        with ExitStack() as es:
            def sb(name, shape, dt=F32):
                return ctx.tile(es.enter_context(nc.sbuf_tensor(uniq(name), list(shape), dt)), name)

            pb = [ctx.tile(es.enter_context(nc.psum_tensor(uniq("pb%d" % i), [128, 512], F32)), "pb%d" % i) for i in range(8)]

            ctx.dma("sync", ident[:], ident_d.ap())
            ctx.dma("sync", offs_sb[:], offs_d.ap())
            ctx.dma("sync", qkn[:], qkn_d.ap())
            ctx.wait_all("sync")

            def ld(e):
                e.reg_load(reg_tok, offs_sb.h[:1, 0:1])
                e.reg_load(reg_x, offs_sb.h[:1, 1:2])
                resnap(e)
            ctx.raw("sync", ld)
            k.cp(identb[:], ident[:])
            k.memset(ones_f[:], 1.0)
            k.memset(ones_b[:], 1.0)

            g_win = cast_gather("win", GQ)
            g_wo = cast_gather("wo", G8)
            g_wpq = cast_gather("wpq", G8)

            cT = sb("cT", [128, KC, 4])
            sg = sb("sg", [128, KC, 4])
            ctx.dma("sync", cT[:], cT_d.ap())
            k.act(sg[:], cT[:], AF.Sigmoid)
            k.tt(cT[:], cT[:], sg[:], ALU.mult)
            wring = Ring([sb("wad%d" % i, [128, KC * 128]) for i in range(3)])
            ncc = 6 * KC
            for ccg in range(ncc):
                wt = wring.next()
                ctx.dma("sync", wt[:], wada_d.ap()[ccg])
                bank = pb[(ccg * 4) // 512]
                o = (ccg * 4) % 512
                for kc in range(KC):
                    k.mm(bank[:, o:o + 4], wt[:, kc * 128:(kc + 1) * 128], cT[:, kc, :], start=(kc == 0), stop=(kc == KC - 1))
            modp = sb("modp_s", [128, ncc * 4])
            for bi in range((ncc * 4 + 511) // 512):
                w = min(512, ncc * 4 - bi * 512)
                k.cp(modp[:, bi * 512: bi * 512 + w], pb[bi][:, 0:w])
            g_win()
            g_wo()
            g_wpq()
            g_uT = cast_gather("uT", G8)
            g_ev = cast_gather("ev", G8)
            modr = modp[:].re("p (c b) -> p c b", b=4)
            oh = sb("oh", [128, 4])
            ctx.dma("sync", oh[:], oh_d.ap())
            bada = sb("bada", [128, ncc])
            ctx.dma("sync", bada[:], badaT_d.ap())
            n1g = sb("n1g", [128, KC])
            n2g = sb("n2g", [128, KC])
            ctx.dma("sync", n1g[:], n1g_d.ap())
            ctx.dma("sync", n2g[:], n2g_d.ap())
            k.ts(msel[:], modr[:, :, 0], oh[:, 0:1], ALU.mult)
            for b in range(1, 4):
                k.stt(msel[:], modr[:, :, b], oh[:, b:b + 1], msel[:], ALU.mult, ALU.add)
            k.tt(msel[:], msel[:], bada[:], ALU.add)
            k.stt(gm1[:], msel[:, KC:2 * KC], 1.0, n1g[:], ALU.add, ALU.mult)
            k.stt(gm2[:], msel[:, 4 * KC:5 * KC], 1.0, n2g[:], ALU.add, ALU.mult)
            sh1 = msel[:, 0:KC]
            for i, o in enumerate((2 * KC, 5 * KC)):
                k.tr(pb[7][0:KC, i * 128:(i + 1) * 128], msel[:, o:o + KC], ident[:])
            gtmp = sb("gtmp", [KC, 256])
            k.cp(gtmp[:], pb[7][0:KC, 0:256])
            ctx.dma("sync", gvec_d.v(gvec_d.h.ap()[0:KC, :]), gtmp[:, 0:128])
            ctx.dma("sync", gvec_d.v(gvec_d.h.ap()[KC:2 * KC, :]), gtmp[:, 128:256])
            k.cp(qkg[:], qkn[:])
            k.ts(qkg[:, 0:1], qkn[:, 0:1], 128.0 ** -0.5, ALU.mult)
            k.ts(qkg[:, 2:3], qkn[:, 2:3], 128.0 ** -0.5, ALU.mult)
            gg = sb("gg", [128, 2])
            k.tt(gg[:, 0:1], qkn[:, 0:1], qkn[:, 1:2], ALU.mult)
            k.tt(gg[:, 1:2], qkn[:, 2:3], qkn[:, 3:4], ALU.mult)
            k.tr(pb[6][0:2, 0:128], gg[:], ident[:])
            ggT = sb("ggT", [2, 128])
            k.cp(ggT[:], pb[6][0:2, 0:128])
            bnd = sb("bnd", [2, 2])
            k.rmax_abs(bnd[:, 0:1], ggT[:])
            relb = sb("relb", [1, 32 * cfg["HA"]])
            ctx.dma("sync", relb[:], relb_d.ap())
            rbm = sb("rbm", [1, 1])
            k.rmax_abs(rbm[:], relb[:])
            k.ts(bnd[:, 0:1], bnd[:, 0:1], -(128.0 ** 0.5), ALU.mult)
            k.ts(rbm[:], rbm[:], -1.0, ALU.mult)
            k.tt(bnd[0:1, 0:1], bnd[0:1, 0:1], rbm[:], ALU.add)
            sel2 = sb("sel2", [2, 2])
            k.ts(sel2[:], ident[0:2, 0:2], bnd[:, 0:1], ALU.mult)
            k.mm(pb[6][:, 256:258], ones_f[0:2, :], sel2[:])
            k.cp(nbound[:], pb[6][:, 256:258])

            NT = S // 128
            xring = Ring([sb("xt%d" % i, [128, D]) for i in range(2)])
            sqj = sb("sqj", [128, D], BF16)
            hblk = Ring([sb("hblk%d" % i, [128, KC, 512], BF16) for i in range(2)])
            st = Ring([sb("st%d" % i, [128, 4]) for i in range(4)])
            pr = Ring(pb[0:6])
            hb = None
            for tt_ in range(NT):
                if tt_ % 4 == 0:
                    hb = hblk.next()
                xt = xring.next()
                ctx.dma("sync", xt[:], xf_d.ap()[tt_ * 128:(tt_ + 1) * 128, :])
                s_ = st.next()
                k.act(sqj[:], xt[:], AF.Square, accum=s_[:, 0:1])
                k.act(s_[:, 1:2], s_[:, 0:1], AF.Ln, bias=EPS, scale=1.0 / D)
                k.act(s_[:, 2:3], s_[:, 1:2], AF.Exp, scale=-0.5)
                k.ts(xt[:], xt[:], s_[:, 2:3], ALU.mult)
                for cg in range(KC // 4):
                    bank = pr.next()
                    for j in range(4):
                        c_ = cg * 4 + j
                        k.tr(bank[:, j * 128:(j + 1) * 128], xt[:, c_ * 128:(c_ + 1) * 128], ident[:])
                    for j in range(4):
                        c_ = cg * 4 + j
                        dst = hb[:, c_, (tt_ % 4) * 128:(tt_ % 4 + 1) * 128]
                        if j % 2 == 0:
                            k.ts(dst, bank[:, j * 128:(j + 1) * 128], gm1[:, c_:c_ + 1], ALU.mult, sh1[:, c_:c_ + 1], ALU.add)
                        else:
                            k.act(dst, bank[:, j * 128:(j + 1) * 128], AF.Identity, bias=sh1[:, c_:c_ + 1], scale=gm1[:, c_:c_ + 1])
                if tt_ % 4 == 3:
                    for sub in range(2):
                        tbg = (tt_ // 4) * 2 + sub
                        half, tbl = tbg // (SH // 256), tbg % (SH // 256)
                        for kq in range(4):
                            row = ((tbl * 4 + kq) * 2 + half) * 128
                            ctx.dma("sync", hTg_d.v(hTg_d.h.ap()[row:row + 128, :].rearrange("p (c t) -> p c t", t=256)),
                                    hb[:, kq * KQ:(kq + 1) * KQ, sub * 256:(sub + 1) * 256])
            if dbg:
                dm = dbgout("msel", [128, 6 * KC])
                ctx.dma("sync", dm.ap(), msel[:])
                dnb = dbgout("nbound", [128, 2])
                ctx.dma("sync", dnb.ap(), nbound[:])
                dh = dbgout("hTg", [NPC * 2 * 128, KQ * 256], BF16)
                ctx.dma("sync", dh.ap(), hTg_d.v(hTg_d.h.ap()))
            ctx.wait_all("sync")
            with nc.Block() as block:
                ctx.flush(block)

        if stop_after == 1:
            return nc, dbg_out

        NT = S // 128
        PBK = 256
        NPB = S // PBK

        def load_hblock(dst, tb):
            half, tbl = tb // (SH // 256), tb % (SH // 256)
            for kq in range(4):
                row = ((tbl * 4 + kq) * 2 + half) * 128
                ctx.dma("sync", dst[:, kq * KQ:(kq + 1) * KQ, :],
                        hTg_d.v(hTg_d.h.ap()[row:row + 128, :].rearrange("p (c t) -> p c t", t=256)))

        for phase in cfg.get("phases", ("A", "B")):
            with ExitStack() as es:
                def sb(name, shape, dt=F32):
                    return ctx.tile(es.enter_context(nc.sbuf_tensor(uniq(name), list(shape), dt)), name)

                pb = [ctx.tile(es.enter_context(nc.psum_tensor(uniq("pb%d" % i), [128, 512], F32)), "pb%d" % i) for i in range(8)]
                Wh = sb("Wh", [128, KC * 384], BF16)
                hring = Ring([sb("hTb%d" % i, [128, KC, PBK], BF16) for i in range(2)])
                QT = sb("QT", [128, S], BF16)
                KT = sb("KT", [128, S], BF16)
                VT = sb("VT", [128, S], BF16)
                sqr = Ring([sb("sqf%d" % i, [128, PBK]) for i in range(2)])
                rsr = Ring([sb("rs%d" % i, [128, PBK]) for i in range(2)])
                tmpr = Ring([sb("tmp%d" % i, [128, 512]) for i in range(3)])
                ptr = Ring([sb("PT%d" % i, [128, 512], BF16) for i in range(3)])
                mixT = sb("mixT", [128, S], BF16)
                sring = Ring([pb[4], pb[5], pb[0], pb[1]])
                numb, denb = pb[6], pb[7]
                misc = Ring([pb[2], pb[3]])
                nb_col = 0 if phase == "A" else 1
                gq = qkg[:, 0:1] if phase == "A" else qkg[:, 2:3]
                gk = qkg[:, 1:2] if phase == "A" else qkg[:, 3:4]
                nbnd = nbound[:, nb_col:nb_col + 1]

                if phase == "A":
                    Vd = [sb("Vd%d" % i, [128, NT, 128], BF16) for i in range(3)]
                    RNG = min(2048, S)
                    numA = sb("numA", [128, RNG])
                    denA = sb("denA", [128, RNG])
                    t1 = sb("t1", [128, RNG])
                    btr = Ring([sb("bt%d" % i, [128, 256]) for i in range(2)])
                    bnt = [sb("bn%d" % i, [128, 512]) for i in range(3)]
                    bft = [sb("bf%d" % i, [128, 512]) for i in range(3)]
                    maskn = sb("maskn", [128, 512])
                    maskf = sb("maskf", [128, 512])
                    ctx.dma("sync", maskn[:], maskn_d.ap())
                    ctx.dma("sync", maskf[:], maskf_d.ap())
                else:
                    Vtm = sb("Vtm", [128, NT, 128], BF16)
                    maskd = sb("maskd", [128, 128])
                    ctx.dma("sync", maskd[:], maskd_d.ap())
                    CS = sb("CS", [nB, S])
                    lg = sb("lg", [nB, S])
                    efr = Ring([sb("ef%d" % i, [nB, PBK]) for i in range(2)])
                    negF = sb("negF", [128, NT * nB])
                    fqr = Ring([sb("fqb%d" % i, [128, 512]) for i in range(2)])
                    sel_all = sb("sel_all", [nB, nB, 128])
                    Wfz = sb("Wfz", [128, KC * nB], BF16)
                    nbf = sb("nbf", [nB, 1])
                    ctx.dma("sync", Wfz[:], wg["win"].dap(NL * 128 * KC * 384, [[KC * nB, 128], [1, KC * nB]]))
                    ctx.dma("sync", nbf[:], bf_d.ap())
                    k.ts(nbf[:], nbf[:], -1.0, ALU.mult)
                    for h_ in range(nB):
                        k.cp(sel_all[:, h_, :], ident[0:nB, h_:h_ + 1].bc([nB, 128]))
                    for tb in range(NPB):
                        hb = hring.next()
                        load_hblock(hb, tb)
                        bank = misc.next()
                        for c_ in range(KC):
                            k.mm(bank[0:nB, 0:PBK], Wfz[:, c_ * nB:(c_ + 1) * nB], hb[:, c_, :], start=(c_ == 0), stop=(c_ == KC - 1))
                        ef = efr.next()
                        k.act(ef[:], bank[0:nB, 0:PBK], AF.Exp, bias=nbf[:, 0:1], scale=-1.0)
                        k.act(lg[:, tb * PBK:(tb + 1) * PBK], ef[:], AF.Ln, bias=1.0, scale=1.0)
                    ctx.op("vector", lambda e, o=CS.h[:], d=lg.h[:]:
                           e.tensor_tensor_scan(out=o, data0=d, data1=d, initial=0.0, op0=ALU.add, op1=ALU.bypass), [lg], [CS])
                    bank = misc.next()
                    for t_ in range(NT):
                        k.tr(bank[:, t_ * nB:(t_ + 1) * nB], CS[:, t_ * 128:(t_ + 1) * 128], ident[0:nB, 0:nB])
                    k.ts(negF[:], bank[:, 0:NT * nB], nbnd, ALU.add)
                    if dbg:
                        dcs = dbgout("CS", [nB, S])
                        ctx.dma("sync", dcs.ap(), CS[:])

                nh = nA if phase == "A" else nB
                for hl in range(nh):
                    slot = hl if phase == "A" else nA + hl
                    ctx.dma("sync", Wh[:], wg["win"].dap(slot * 128 * KC * 384, [[KC * 384, 128], [1, KC * 384]]))
                    for tb in range(NPB):
                        hb = hring.next()
                        load_hblock(hb, tb)
                        cols = slice(tb * PBK, (tb + 1) * PBK)
                        for j in range(3):
                            for c_ in range(KC):
                                k.mm(pb[j][:, 0:PBK], Wh[:, c_ * 384 + j * 128: c_ * 384 + (j + 1) * 128], hb[:, c_, :],
                                     start=(c_ == 0), stop=(c_ == KC - 1))
                        k.cp(VT[:, cols], pb[2][:, 0:PBK], eng="scalar")
                        for j, (dst, gn) in enumerate(((QT, gq), (KT, gk))):
                            sq = sqr.next()
                            rs = rsr.next()
                            k.act(sq[:], pb[j][:, 0:PBK], AF.Square)
                            k.mm(pb[3][:, j * PBK:(j + 1) * PBK], ones_f[:], sq[:])
                            k.act(rs[:], pb[3][:, j * PBK:(j + 1) * PBK], AF.Ln, bias=EPS, scale=1.0 / 128)
                            k.act(rs[:], rs[:], AF.Exp, scale=-0.5)
                            k.stt(dst[:, cols], pb[j][:, 0:PBK], gn, rs[:], ALU.mult, ALU.mult)

                    if cfg.get("lvl", 9) < 1:
                        continue
                    if phase == "A":
                        for br, (w_, d_) in enumerate(cfg["BR"]):
                            nblk = S // (128 * d_)
                            for b0 in range(0, NT, 4):
                                bank = misc.next()
                                bv = bank[:].bitcast(BF16)
                                for j in range(4):
                                    blk = b0 + j
                                    r_, n_ = blk // nblk, blk % nblk
                                    st_ = r_ + d_ * n_ * 128
                                    k.tr(bv[:, j * 128:(j + 1) * 128], VT[:, st_: st_ + d_ * 127 + 1: d_], identb[:])
                                k.cp(Vd[br][:, b0:b0 + 4, :].re("p a b -> p (a b)"), bv[:, 0:512], eng=("scalar" if (b0 // 4) % 2 else "vector"))
                            bt = btr.next()
                            ctx.dma("sync", bt[:], biasT_d.ap()[hl, br])
                            for hf in range(2):
                                k.tt(bnt[br][:, hf * 256:(hf + 1) * 256], bt[:], maskn[:, hf * 256:(hf + 1) * 256], ALU.add)
                                k.tt(bft[br][:, hf * 256:(hf + 1) * 256], bt[:], maskf[:, hf * 256:(hf + 1) * 256], ALU.add)
                        for rg in range(S // RNG if cfg.get("lvl", 9) >= 2 else 0):
                            for br, (w_, d_) in enumerate(cfg["BR"]):
                                nblk = S // (128 * d_)
                                per = RNG // (128 * d_)
                                for r_ in range(d_):
                                    n_lo, n_hi = rg * per, (rg + 1) * per
                                    for n0 in range(n_lo, n_hi, 2):
                                        nq = min(2, n_hi - n0)
                                        sp = sring.next()

                                        def tk_(n_):
                                            st_ = r_ + d_ * n_ * 128
                                            return slice(st_, st_ + d_ * 127 + 1, d_)
                                        for qi in range(nq):
                                            n_ = n0 + qi
                                            np_ = max(n_ - 1, 0)
                                            k.mm(sp[:, qi * 256: qi * 256 + 128], KT[:, tk_(np_)], QT[:, tk_(n_)])
                                            k.mm(sp[:, qi * 256 + 128: qi * 256 + 256], KT[:, tk_(n_)], QT[:, tk_(n_)])
                                        W_ = nq * 256
                                        tmp = tmpr.next()
                                        PT = ptr.next()
                                        bias_t = bft[br] if n0 == 0 else bnt[br]
                                        k.tt(tmp[:, 0:W_], sp[:, 0:W_], bias_t[:, 0:W_], ALU.add)
                                        k.act(PT[:, 0:W_], tmp[:, 0:W_], AF.Exp, bias=nbnd, scale=1.0)
                                        for qi in range(nq):
                                            n_ = n0 + qi
                                            np_ = max(n_ - 1, 0)
                                            o_ = slice(qi * 128, (qi + 1) * 128)
                                            k.mm(numb[:, o_], Vd[br][:, r_ * nblk + np_, :], PT[:, qi * 256: qi * 256 + 128], start=True, stop=False)
                                            k.mm(numb[:, o_], Vd[br][:, r_ * nblk + n_, :], PT[:, qi * 256 + 128: qi * 256 + 256], start=False, stop=True)
                                        for qi in range(nq):
                                            o_ = slice(qi * 128, (qi + 1) * 128)
                                            k.mm(denb[:, o_], ones_b[:], PT[:, qi * 256: qi * 256 + 128], start=True, stop=False)
                                            k.mm(denb[:, o_], ones_b[:], PT[:, qi * 256 + 128: qi * 256 + 256], start=False, stop=True)
                                        st_ = r_ + d_ * n0 * 128 - rg * RNG
                                        dsl = slice(st_, st_ + d_ * (nq * 128 - 1) + 1, d_)
                                        if br == 0:
                                            k.cp(numA[:, dsl], numb[:, 0:nq * 128], eng="scalar")
                                            k.cp(denA[:, dsl], denb[:, 0:nq * 128], eng="vector")
                                        else:
                                            k.tt(numA[:, dsl], numA[:, dsl], numb[:, 0:nq * 128], ALU.add)
                                            k.tt(denA[:, dsl], denA[:, dsl], denb[:, 0:nq * 128], ALU.add)
                            k.act(t1[:], denA[:], AF.Ln)
                            k.act(t1[:], t1[:], AF.Exp, scale=-1.0)
                            k.tt(mixT[:, rg * RNG:(rg + 1) * RNG], numA[:], t1[:], ALU.mult)
                    else:
                        for b0 in range(0, NT, 4):
                            bank = misc.next()
                            bv = bank[:].bitcast(BF16)
                            for j in range(4):
                                k.tr(bv[:, j * 128:(j + 1) * 128], VT[:, (b0 + j) * 128:(b0 + j + 1) * 128], identb[:])
                            k.cp(Vtm[:, b0:b0 + 4, :].re("p a b -> p (a b)"), bv[:, 0:512], eng=("scalar" if (b0 // 4) % 2 else "vector"))
                        QG = 512
                        for g_ in range(S // QG):
                            bank = misc.next()
                            k.mm(bank[:, 0:QG], sel_all[:, hl, :], CS[:, g_ * QG:(g_ + 1) * QG])
                            fqb = fqr.next()
                            k.cp(fqb[:], bank[:, 0:QG], eng="scalar")
                            nj = 4 * g_ + 4
                            sps = {}
                            sps[0] = sring.next()
                            k.mm(sps[0][:, 0:QG], KT[:, 0:128], QT[:, g_ * QG:(g_ + 1) * QG])
                            for j in range(nj):
                                if j + 1 < nj:
                                    sps[j + 1] = sring.next()
                                    c1 = max(0, j + 1 - 4 * g_) * 128
                                    k.mm(sps[j + 1][:, c1:QG], KT[:, (j + 1) * 128:(j + 2) * 128], QT[:, g_ * QG + c1:(g_ + 1) * QG])
                                sp = sps.pop(j)
                                m_ = j - 4 * g_
                                c0 = max(0, m_) * 128
                                tmp = tmpr.next()
                                PT = ptr.next()
                                k.tt(tmp[:, c0:QG], sp[:, c0:QG], fqb[:, c0:QG], ALU.subtract)
                                if m_ >= 0:
                                    k.tt(tmp[:, c0:c0 + 128], tmp[:, c0:c0 + 128], maskd[:], ALU.add)
                                k.act(PT[:, c0:QG], tmp[:, c0:QG], AF.Exp, bias=negF[:, j * nB + hl: j * nB + hl + 1], scale=1.0)
                                k.mm(numb[:, c0:QG], Vtm[:, j, :], PT[:, c0:QG], start=(j == 0), stop=(j == nj - 1))
                                k.mm(denb[:, c0:QG], ones_b[:], PT[:, c0:QG], start=(j == 0), stop=(j == nj - 1))
                            tl = tmpr.next()
                            k.act(tl[:, 0:QG], denb[:, 0:QG], AF.Ln)
                            k.act(tl[:, 0:QG], tl[:, 0:QG], AF.Exp, scale=-1.0)
                            k.tt(mixT[:, g_ * QG:(g_ + 1) * QG], numb[:, 0:QG], tl[:, 0:QG], ALU.mult)
                    for th in range(2):
                        pc = slot * 2 + th
                        ctx.dma("sync", mixg_d.v(mixg_d.h.ap()[pc * 128:(pc + 1) * 128, :]), mixT[:, th * SH:(th + 1) * SH])
                if phase == "B":
                    if dbg:
                        dmx = dbgout("mixl", [NL * 2 * 128, SH], BF16)
                        ctx.dma("sync", dmx.ap(), mixg_d.v(mixg_d.h.ap()))
                ctx.wait_all("sync")
                with nc.Block() as block:
                    ctx.raw("sync", resnap)
                    ctx.flush(block)
        if stop_after == 2:
            return nc, dbg_out

        def norm_tile(xt, xn, hb, col0, gm, sh, sqj, s_, prng):
            k.act(sqj[:], xt[:], AF.Square, accum=s_[:, 0:1])
            k.act(s_[:, 1:2], s_[:, 0:1], AF.Ln, bias=EPS, scale=1.0 / D)
            k.act(s_[:, 2:3], s_[:, 1:2], AF.Exp, scale=-0.5)
            k.ts(xn[:], xt[:], s_[:, 2:3], ALU.mult)
            for cg in range(KC // 4):
                bank = prng.next()
                for j in range(4):
                    c_ = cg * 4 + j
                    k.tr(bank[:, j * 128:(j + 1) * 128], xn[:, c_ * 128:(c_ + 1) * 128], ident[:])
                for j in range(4):
                    c_ = cg * 4 + j
                    dst = hb[:, c_, col0:col0 + 128]
                    if j % 2 == 0:
                        k.ts(dst, bank[:, j * 128:(j + 1) * 128], gm[:, c_:c_ + 1], ALU.mult, sh[:, c_:c_ + 1], ALU.add)
                    else:
                        k.act(dst, bank[:, j * 128:(j + 1) * 128], AF.Identity, bias=sh[:, c_:c_ + 1], scale=gm[:, c_:c_ + 1])

        with ExitStack() as es:
            def sb(name, shape, dt=F32):
                return ctx.tile(es.enter_context(nc.sbuf_tensor(uniq(name), list(shape), dt)), name)

            pb = [ctx.tile(es.enter_context(nc.psum_tensor(uniq("pb%d" % i), [128, 512], F32)), "pb%d" % i) for i in range(8)]
            g1b = sb("g1b", [128, D])
            ctx.dma("sync", g1b[:], gvec_d.dap(0, [[0, 128], [1, D]]))
            sh2 = msel[:, 3 * KC:4 * KC]
            mring = Ring([sb("mixb%d" % i, [128, KC, 256], BF16) for i in range(2)])
            wring = Ring([sb("wo%d" % i, [128, KC * 256], BF16) for i in range(2)])
            ytl = [sb("y%d" % i, [128, D]) for i in range(2)]
            xring = Ring([sb("xr%d" % i, [128, D]) for i in range(2)])
            sqj = sb("sqj", [128, D], BF16)
            h2b = Ring([sb("h2b%d" % i, [128, KC, 256], BF16) for i in range(2)])
            st = Ring([sb("st%d" % i, [128, 4]) for i in range(4)])
            pyr = Ring(pb[0:4])
            ptr_ = Ring(pb[4:8])
            mdims = [[SH, 128], [2 * 128 * SH, KC], [1, 256]]
            for tb in range(SH // 256):
                mb = mring.next()
                ctx.dma("sync", mb[:], mixg_d.dap(0, mdims),
                        in_fn=lambda tb=tb: bass.AP(mixg_d.h, ctx.dyn["tok"] + tb * 256, [list(d) for d in mdims]))
                for ng in range(D // 256):
                    wt = wring.next()
                    ctx.dma("sync", wt[:], wg["wo"].dap(ng * 128 * KC * 256, [[KC * 256, 128], [1, KC * 256]]))
                    for tt in range(2):
                        bank = pyr.next()
                        for c_ in range(KC):
                            k.mm(bank[:, 0:256], mb[:, c_, tt * 128:(tt + 1) * 128], wt[:, c_ * 256:(c_ + 1) * 256],
                                 start=(c_ == 0), stop=(c_ == KC - 1))
                        k.tt(ytl[tt][:, ng * 256:(ng + 1) * 256], bank[:, 0:256], g1b[:, ng * 256:(ng + 1) * 256], ALU.mult)
                hb = h2b.next()
                for tt in range(2):
                    ti = tb * 2 + tt
                    xr = xring.next()
                    ctx.dma("sync", xr[:], xh_d.ap()[ti * 128:(ti + 1) * 128, :])
                    y = ytl[tt]
                    k.tt(y[:], y[:], xr[:], ALU.add)
                    ctx.dma("sync", x1_d.v(x1_d.h.ap()[ti * 128:(ti + 1) * 128, :]), y[:])
                    norm_tile(y, xr, hb, tt * 128, gm2, sh2, sqj, st.next(), ptr_)
                ctx.dma("sync", h2T_d.dap(tb * 256, [[KC * SH, 128], [SH, KC], [1, 256]]), hb[:])
            if dbg:
                d1 = dbgout("x1", [SH, D])
                ctx.dma("sync", d1.ap(), x1_d.v(x1_d.h.ap()))
                d2 = dbgout("h2T", [128, KC * SH], BF16)
                ctx.dma("sync", d2.ap(), h2T_d.v(h2T_d.h.ap()))
            ctx.wait_all("sync")
            with nc.Block() as block:
                ctx.raw("sync", resnap)
                ctx.ops["sync"].insert(0, ctx.ops["sync"].pop())
                ctx.flush(block)
        if stop_after == 3:
            return nc, dbg_out

        with ExitStack() as es:
            def sb(name, shape, dt=F32):
                return ctx.tile(es.enter_context(nc.sbuf_tensor(uniq(name), list(shape), dt)), name)

            pbh = [es.enter_context(nc.psum_tensor(uniq("pb%d" % i), [128, 512], F32)) for i in range(8)]
            pb = [ctx.tile(h_, "pb%d" % i) for i, h_ in enumerate(pbh)]
            gring = Ring([pb[6], pb[7]])
            g2 = msel[:, 5 * KC:6 * KC]
            skf = sb("skf", [128, 2, 128])
            skb = sb("skb", [128, 2, 128], BF16)
            ctx.dma("sync", skf[:], skT_d.ap())
            k.cp(skb[:], skf[:])
            h2r = Ring([sb("h2r%d" % i, [128, KC, 256], BF16) for i in range(1)])
            wqr = Ring([sb("wq%d" % i, [128, KC * 128], BF16) for i in range(2)])
            qT = sb("qT", [128, 16, 256], BF16)
            S12 = [sb("S12_%d" % i, [128, 16, 128]) for i in range(2)]
            thr = [sb("thr%d" % i, [128, 8]) for i in range(2)]
            b3 = [sb("b3_%d" % i, [128, 8]) for i in range(2)]
            v1r = Ring([sb("v1_%d" % i, [128, 16]) for i in range(2)])
            v2r = Ring([sb("v2_%d" % i, [128, 16]) for i in range(2)])
            vsr = Ring([sb("vs_%d" % i, [128, 16]) for i in range(2)])
            smr = Ring([sb("sm_%d" % i, [128, 128]) for i in range(2)])
            cdr = Ring([sb("cd_%d" % i, [128, 256]) for i in range(2)])
            cmr = Ring([sb("cm_%d" % i, [128, 256]) for i in range(2)])
            zr = Ring([sb("z_%d" % i, [128, 4]) for i in range(2)])
            j16 = sb("j16", [128, 16])
            tmpa = Ring([sb("tmpa%d" % i, [128, 1024]) for i in range(2)])
            tmpe = Ring([sb("tmpe%d" % i, [128, 1024]) for i in range(2)])
            tmp2 = Ring([sb("tmp2%d" % i, [128, 1024], BF16) for i in range(2)])
            Ggr = [Ring([sb("Gg%d_%d" % (t_, i), [128, 2048], BF16) for i in range(2)]) for t_ in range(2)]
            Ur = Ring([sb("U%d" % i, [128, KC * 128], BF16) for i in range(3)])
            gel = Ring([sb("gel%d" % i, [128, 256]) for i in range(3)])
            aTr = Ring([sb("aT%d" % i, [128, 16, 256], BF16) for i in range(2)])
            Vr = Ring([sb("Vs%d" % i, [128, 512], BF16) for i in range(4)])
            yacc = sb("yacc", [128, KC, 256])
            x1r = Ring([sb("x1p%d" % i, [128, 512]) for i in range(3)])
            aring = Ring([pb[0], pb[1]])
            accb = pb[2:6]
            NEG_ = NEG
            for tb in range(SH // 256):
                h2 = h2r.next()
                ctx.dma("sync", h2[:], h2T_d.dap(tb * 256, [[KC * SH, 128], [SH, KC], [1, 256]]))
                for jc in range(16):
                    wq = wqr.next()
                    ctx.dma("sync", wq[:], wg["wpq"].dap(jc * 128 * KC * 128, [[KC * 128, 128], [1, KC * 128]]))
                    bank = aring.next()
                    for c_ in range(KC):
                        k.mm(bank[:, 0:256], wq[:, c_ * 128:(c_ + 1) * 128], h2[:, c_, :], start=(c_ == 0), stop=(c_ == KC - 1))
                    k.cp(qT[:, jc, :], bank[:, 0:256], eng=("scalar" if jc % 2 else "vector"))
                for tt in range(2):
                    for q4 in range(4):
                        bank = aring.next()
                        for j in range(4):
                            jc = q4 * 4 + j
                            k.mm(bank[:, j * 128:(j + 1) * 128], qT[:, jc, tt * 128:(tt + 1) * 128], skb[:, jc % 2, :])
                        k.cp(S12[tt][:, q4 * 4:(q4 + 1) * 4, :].re("p a b -> p (a b)"), bank[:, 0:512], eng=("scalar" if q4 % 2 else "vector"))
                    for h_ in range(8):
                        s1 = S12[tt][:, 2 * h_, :]
                        s2 = S12[tt][:, 2 * h_ + 1, :]
                        v1, v2, vs = v1r.next(), v2r.next(), vsr.next()
                        for (sv, vv) in ((s1, v1), (s2, v2)):
                            sm = smr.next()
                            k.max8(vv[:, 0:8], sv)
                            k.mrep(sm[:], vv[:, 0:8], sv)
                            k.max8(vv[:, 8:16], sm[:])
                        cd, cm = cdr.next(), cmr.next()
                        k.tt(cd[:].re("p (a b) -> p a b", b=16), v1[:].unsq(2).bc([128, 16, 16]), v2[:].unsq(1).bc([128, 16, 16]), ALU.add)
                        k.max8(vs[:, 0:8], cd[:])
                        k.mrep(cm[:], vs[:, 0:8], cd[:])
                        k.max8(vs[:, 8:16], cm[:])
                        z = zr.next()
                        k.ts(z[:, 0:1], vs[:, 0:1], -1.0, ALU.mult)
                        k.act(j16[:], vs[:], AF.Exp, bias=z[:, 0:1], scale=1.0, accum=z[:, 1:2])
                        k.act(z[:, 2:3], z[:, 1:2], AF.Ln)
                        k.tt(b3[tt][:, h_:h_ + 1], z[:, 0:1], z[:, 2:3], ALU.subtract)
                        k.cp(thr[tt][:, h_:h_ + 1], vs[:, 15:16])
                for eg in range(NE // 2048):
                    Gg = [Ggr[0].next(), Ggr[1].next()]
                    for tt in range(2):
                        for h_ in range(8):
                            s1 = S12[tt][:, 2 * h_, :]
                            s2 = S12[tt][:, 2 * h_ + 1, :]
                            for sub in range(2):
                                e0 = eg * 16 + sub * 8
                                ta, te = tmpa.next(), tmpe.next()
                                k.tt(ta[:].re("p (a b) -> p a b", b=128), s1[:, e0:e0 + 8].unsq(2).bc([128, 8, 128]),
                                     s2.unsq(1).bc([128, 8, 128]), ALU.add)
                                k.act(te[:], ta[:], AF.Exp, bias=b3[tt][:, h_:h_ + 1], scale=1.0)
                                gdst = Gg[tt][:, sub * 1024:(sub + 1) * 1024]
                                if h_ == 0:
                                    k.stt(gdst, ta[:], thr[tt][:, h_:h_ + 1], te[:], ALU.is_ge, ALU.mult)
                                else:
                                    t2 = tmp2.next()
                                    k.stt(t2[:], ta[:], thr[tt][:, h_:h_ + 1], te[:], ALU.is_ge, ALU.mult)
                                    k.tt(gdst, gdst, t2[:], ALU.add, eng="gpsimd")
                    aT = aTr.next()
                    for ec in range(16):
                        U = Ur.next()
                        ctx.dma("sync", U[:], wg["uT"].dap((eg * 16 + ec) * 128 * KC * 128, [[KC * 128, 128], [1, KC * 128]]))
                        gs = gring.next()
                        gv = gs[:].bitcast(BF16)[:, 0:256]
                        for tt in range(2):
                            k.tr(gv[:, tt * 128:(tt + 1) * 128], Gg[tt][:, ec * 128:(ec + 1) * 128], identb[:])
                        bank = aring.next()
                        for c_ in range(KC):
                            k.mm(bank[:, 0:256], U[:, c_ * 128:(c_ + 1) * 128], h2[:, c_, :], start=(c_ == 0), stop=(c_ == KC - 1))
                        ge = gel.next()
                        k.act(ge[:], bank[:, 0:256], AF.Gelu)
                        k.tt(aT[:, ec, :], ge[:], gv, ALU.mult)
                    for dg in range(KC // 4):
                        for ec in range(16):
                            Vt = Vr.next()
                            ctx.dma("sync", Vt[:], wg["ev"].dap(((eg * 16 + ec) * 128) * D + dg * 512, [[D, 128], [1, 512]]))
                            for i in range(4):
                                k.mm(accb[i][:, 0:256], Vt[:, i * 128:(i + 1) * 128], aT[:, ec, :], start=(ec == 0), stop=(ec == 15))
                        for i in range(4):
                            dc = dg * 4 + i
                            if eg == 0:
                                k.ts(yacc[:, dc, :], accb[i][:, 0:256], g2[:, dc:dc + 1], ALU.mult)
                            else:
                                k.stt(yacc[:, dc, :], accb[i][:, 0:256], g2[:, dc:dc + 1], yacc[:, dc, :], ALU.mult, ALU.add)
                for tt in range(2):
                    ti = tb * 2 + tt
                    for cg in range(KC // 4):
                        bank = aring.next()
                        for j in range(4):
                            k.tr(bank[:, j * 128:(j + 1) * 128], yacc[:, cg * 4 + j, tt * 128:(tt + 1) * 128], ident[:])
                        xp = x1r.next()
                        ctx.dma("sync", xp[:], x1_d.v(x1_d.h.ap()[ti * 128:(ti + 1) * 128, cg * 512:(cg + 1) * 512]))
                        k.tt(xp[:], xp[:], bank[:, 0:512], ALU.add)
                        ctx.dma("sync", out_d.ap()[ti * 128:(ti + 1) * 128, cg * 512:(cg + 1) * 512], xp[:])
            ctx.wait_all("sync")
            with nc.Block() as block:
                ctx.flush(block)
    return nc, dbg_out


_CACHE = {}


def kernel(**inputs):
    D = int(np.asarray(inputs["x"]).shape[2])
    S = int(np.asarray(inputs["x"]).shape[1])
    cfg = make_cfg(D, S)
    maps = host_prep(inputs, cfg)
    key = (D, S)
    if key not in _CACHE:
        _CACHE[key] = build(cfg)[0]
    nc = _CACHE[key]
    res = run_bass_kernel_spmd(nc, maps, core_ids=list(range(8))).results
    SH = S // 2
    out = np.empty((4, S, D), np.float32)
    for r in range(8):
        b, g = r // 2, r % 2
        out[b, g * SH:(g + 1) * SH] = np.asarray(res[r]["out"], np.float32)
    return out
```

```python
import math
from contextlib import ExitStack
import numpy as np
import concourse.bass as bass
import concourse.mybir as mybir
from concourse.bass_utils import run_bass_kernel_spmd

F32 = mybir.dt.float32
BF16 = mybir.dt.bfloat16
I32 = mybir.dt.int32
AF = mybir.ActivationFunctionType
ALU = mybir.AluOpType

ENGS = ("tensor", "vector", "scalar", "gpsimd", "sync")
NEG = -1e30
EPS = 1e-6


class Tile:
    def __init__(self, ctx, h, name):
        self.ctx = ctx
        self.h = h
        self.name = name
        self.w = []
        self.r = []
        self.dsem = None
        self.dram = False

    def __getitem__(self, idx):
        return V(self, self.h[idx])

    def v(self, ap):
        return V(self, ap)

    def dap(self, off, dims):
        return V(self, bass.AP(self.h, off, [list(d) for d in dims]))


class V:
    def __init__(self, tile, ap):
        self.t = tile
        self.ap = ap

    def __getitem__(self, idx):
        return V(self.t, self.ap[idx])

    def re(self, s, **kw):
        return V(self.t, self.ap.rearrange(s, **kw))

    def bc(self, shape):
        return V(self.t, self.ap.broadcast_to(list(shape)))

    def unsq(self, ax):
        return V(self.t, self.ap.unsqueeze(ax))

    def bitcast(self, dt):
        return V(self.t, self.ap.bitcast(dt))


def _ap(x):
    return x.ap if isinstance(x, V) else x


class Ctx:
    def __init__(self, nc):
        self.nc = nc
        self.sems = {}
        self.semval = {}
        self.ops = {e: [] for e in ENGS}
        self.waited = {e: {} for e in ENGS}
        self.ninst = {e: 0 for e in ENGS}
        self.dyn = {}
        for e in ENGS:
            self._newsem("E_" + e)

    def _newsem(self, key):
        self.sems[key] = self.nc.alloc_semaphore(name=key)
        self.semval[key] = 0
        return key

    def tile(self, h, name):
        return Tile(self, h, name)

    def _deps(self, eng, reads, writes, is_dma=False):
        ev = []
        for t in reads:
            if t is not None:
                ev.extend(t.w)
        for t in writes:
            if t is None:
                continue
            for (k, v, e) in t.w:
                if is_dma and e == "dma":
                    continue
                if (not is_dma) and e == eng and eng == "tensor":
                    continue
                ev.append((k, v, e))
            for (k, v, e) in t.r:
                if (not is_dma) and e == eng and eng == "tensor":
                    continue
                ev.append((k, v, e))
        need = {}
        for (k, v, e) in ev:
            if v > need.get(k, 0):
                need[k] = v
        out = []
        w = self.waited[eng]
        for k, v in need.items():
            if w.get(k, 0) >= v:
                continue
            w[k] = v
            out.append((k, v))
        return out

    def _commit(self, reads, writes, event):
        for t in writes:
            if t is None:
                continue
            if event[2] == "dma":
                t.w = [x for x in t.w if x[2] == "dma" and x[0] != event[0]] + [event]
            else:
                t.w = [event]
            t.r = []
        for t in reads:
            if t is None or t in writes:
                continue
            t.r = [x for x in t.r if x[0] != event[0]] + [event]

    def op(self, eng, fn, reads=(), writes=()):
        rt = [x.t if isinstance(x, V) else x for x in reads]
        wt = [x.t if isinstance(x, V) else x for x in writes]
        waits = self._deps(eng, rt, wt)
        key = "E_" + eng
        self.semval[key] += 1
        val = self.semval[key]
        sems = self.sems

        def emit(e, waits=waits, fn=fn, key=key):
            for (k, v) in waits:
                e.wait_ge(sems[k], v)
            fn(e).then_inc(sems[key], 1)

        self.ops[eng].append(emit)
        self.ninst[eng] += 1
        self._commit(rt, wt, (key, val, eng))

    def dma(self, eng, out, in_, in_fn=None, **kw):
        ot = out.t if isinstance(out, V) else None
        it = in_.t if isinstance(in_, V) else None
        cands = [t for t in (ot, it) if t is not None]
        sbs = [t for t in cands if not t.dram]
        owner = sbs[0] if sbs else cands[0]
        if owner.dsem is None:
            owner.dsem = self._newsem("D%d_%s" % (len(self.sems), owner.name))
        key = owner.dsem
        waits = self._deps(eng, [it], [ot], is_dma=True)
        self.semval[key] += 16
        val = self.semval[key]
        sems = self.sems
        oap, iap = _ap(out), _ap(in_)

        def emit(e, waits=waits):
            for (k, v) in waits:
                e.wait_ge(sems[k], v)
            src = in_fn() if in_fn is not None else iap
            e.dma_start(out=oap, in_=src, **kw).then_inc(sems[key], 16)

        self.ops[eng].append(emit)
        self.ninst[eng] += 1
        self._commit([it], [ot], (key, val, "dma"))

    def collective(self, kind, op, groups, in_, out):
        eng = "gpsimd"
        it, ot = in_.t, out.t
        if ot.dsem is None:
            ot.dsem = self._newsem("D%d_%s" % (len(self.sems), ot.name))
        key = ot.dsem
        waits = self._deps(eng, [it], [ot], is_dma=True)
        self.semval[key] += 1
        val = self.semval[key]
        sems = self.sems
        oap, iap = _ap(out), _ap(in_)

        def emit(e, waits=waits):
            for (k, v) in waits:
                e.wait_ge(sems[k], v)
            e.collective_compute(kind, op, replica_groups=groups, ins=[iap], outs=[oap]).then_inc(sems[key], 1)

        self.ops[eng].append(emit)
        self._commit([it], [ot], (key, val, "dma"))

    def raw(self, eng, fn):
        self.ops[eng].append(fn)

    def wait_all(self, eng):
        waits = []
        w = self.waited[eng]
        for k, v in self.semval.items():
            if v > 0 and w.get(k, 0) < v:
                w[k] = v
                waits.append((k, v))
        sems = self.sems

        def emit(e, waits=waits):
            for (k, v) in waits:
                e.wait_ge(sems[k], v)

        self.ops[eng].append(emit)

    def flush(self, block):
        for name in ENGS:
            lst = self.ops[name]
            if not lst:
                continue

            def body(e, lst=lst):
                for f in lst:
                    f(e)

            getattr(block, name)(body)
            self.ops[name] = []


class Ring:
    def __init__(self, tiles):
        self.tiles = tiles
        self.i = 0

    def next(self):
        t = self.tiles[self.i % len(self.tiles)]
        self.i += 1
        return t


class K:
    def __init__(self, ctx):
        self.c = ctx

    def mm(self, out, lhsT, rhs, start=True, stop=True):
        self.c.op("tensor", lambda e, o=out.ap, l=lhsT.ap, r=rhs.ap, s=start, p=stop:
                  e.matmul(o, lhsT=l, rhs=r, start=s, stop=p), [lhsT, rhs], [out])

    def tr(self, out, in_, ident):
        self.c.op("tensor", lambda e, o=out.ap, i=in_.ap, d=ident.ap: e.transpose(o, i, d), [in_, ident], [out])

    def act(self, out, in_, func, bias=None, scale=None, accum=None):
        rd = [in_]
        kw = {}
        if bias is not None:
            if isinstance(bias, V):
                rd.append(bias)
                kw["bias"] = bias.ap
            else:
                kw["bias"] = bias
        if scale is not None:
            if isinstance(scale, V):
                rd.append(scale)
                kw["scale"] = scale.ap
            else:
                kw["scale"] = scale
        wr = [out]
        if accum is not None:
            kw["accum_out"] = accum.ap
            wr.append(accum)
        self.c.op("scalar", lambda e, o=out.ap, i=in_.ap, f=func, kw=kw: e.activation(out=o, in_=i, func=f, **kw), rd, wr)

    def ts(self, out, in0, s1, op0, s2=None, op1=None, eng="vector"):
        rd = [in0]
        a1 = s1.ap if isinstance(s1, V) else s1
        a2 = s2.ap if isinstance(s2, V) else s2
        if isinstance(s1, V):
            rd.append(s1)
        if isinstance(s2, V):
            rd.append(s2)
        kw = {}
        if op1 is not None:
            kw["op1"] = op1
        self.c.op(eng, lambda e, o=out.ap, i=in0.ap, a1=a1, a2=a2, op0=op0, kw=kw:
                  e.tensor_scalar(out=o, in0=i, scalar1=a1, scalar2=a2, op0=op0, **kw), rd, [out])

    def tt(self, out, in0, in1, op, eng="vector"):
        self.c.op(eng, lambda e, o=out.ap, a=in0.ap, b=in1.ap, op=op: e.tensor_tensor(out=o, in0=a, in1=b, op=op),
                  [in0, in1], [out])

    def stt(self, out, in0, s, in1, op0, op1):
        rd = [in0, in1]
        a = s.ap if isinstance(s, V) else s
        if isinstance(s, V):
            rd.append(s)
        self.c.op("vector", lambda e, o=out.ap, i0=in0.ap, a=a, i1=in1.ap, op0=op0, op1=op1:
                  e.scalar_tensor_tensor(out=o, in0=i0, scalar=a, in1=i1, op0=op0, op1=op1), rd, [out])

    def cp(self, out, in_, eng="vector"):
        if eng == "scalar":
            self.c.op("scalar", lambda e, o=out.ap, i=in_.ap: e.copy(out=o, in_=i), [in_], [out])
        else:
            self.c.op(eng, lambda e, o=out.ap, i=in_.ap: e.tensor_copy(out=o, in_=i), [in_], [out])

    def max8(self, out, in_):
        self.c.op("vector", lambda e, o=out.ap, i=in_.ap: e.max(out=o, in_=i), [in_], [out])

    def mrep(self, out, rep, vals):
        self.c.op("vector", lambda e, o=out.ap, r=rep.ap, v=vals.ap:
                  e.match_replace(out=o, in_to_replace=r, in_values=v, imm_value=NEG), [rep, vals], [out])

    def scan_add(self, out, ones, data):
        self.c.op("vector", lambda e, o=out.ap, d0=ones.ap, d1=data.ap:
                  e.tensor_tensor_scan(out=o, data0=d0, data1=d1, initial=0.0, op0=ALU.mult, op1=ALU.add),
                  [ones, data], [out])

    def memset(self, out, val, eng="vector"):
        self.c.op(eng, lambda e, o=out.ap, v=val: e.memset(o, v), [], [out])

    def rmax_abs(self, out, in_):
        self.c.op("vector", lambda e, o=out.ap, i=in_.ap:
                  e.tensor_reduce(out=o, in_=i, axis=mybir.AxisListType.X, op=ALU.max, apply_absolute_value=True),
                  [in_], [out])


def _fact(n, cap=2048):
    for c in range(cap, 0, -1):
        if n % c == 0:
            return c
    return 1


def make_cfg(D, S):
    c = dict(D=D, S=S, B=4, KC=D // 128, NH=D // 128)
    c["HA"] = c["HB"] = c["NH"] // 2
    c["nA"] = c["HA"]
    c["nB"] = c["HB"]
    c["KC8"] = D // 8 // 128
    c["NE"] = 16384
    c["PH"] = 8
    c["BR"] = ((128, 1), (512, 4), (2048, 16))
    nl = c["nA"] + c["nB"]
    CH = 128 * 2048

    def pad(n, nr):
        q = nr * CH
        return (n + q - 1) // q * q
    c["CH"] = CH
    c["win_n"] = pad(nl * 128 * c["KC"] * 384 + 128 * c["KC"] * c["nB"], 1)
    c["wo_n"] = pad(D * D, 1)
    c["wpq_n"] = pad(D * 2048, 1)
    c["uT_n"] = pad(c["NE"] * D, 1)
    c["ev_n"] = pad(c["NE"] * D, 1)
    return c


def _t5_bucket(dist):
    max_exact = 16
    d32 = np.maximum(dist, 1).astype(np.float32)
    large = max_exact + (np.log(d32 / max_exact) / math.log(2048 / max_exact) * (32 - max_exact)).astype(np.int32)
    large = np.minimum(large, 31)
    return np.where(dist < max_exact, dist, large)


def host_prep(inp, cfg):
    D, S, KC, KC8 = cfg["D"], cfg["S"], cfg["KC"], cfg["KC8"]
    HA, HB, nA, nB = cfg["HA"], cfg["HB"], cfg["nA"], cfg["nB"]
    WA = HA * 128
    WB = HB * 128
    f32 = np.float32
    x = np.asarray(inp["x"], f32)
    c = np.asarray(inp["c"], f32)
    w_ada = np.asarray(inp["w_ada"], f32)[0]
    b_ada = np.asarray(inp["b_ada"], f32)[0]
    w_in = np.asarray(inp["w_in"], f32)[0]
    w_o = np.asarray(inp["w_o"], f32)[0]
    w_pq = np.asarray(inp["w_pq"], f32)[0]
    eu = np.asarray(inp["expert_u"], f32)[0]
    ev = np.asarray(inp["expert_v"], f32)[0]
    rel_bias = np.asarray(inp["rel_bias"], f32)
    b_f = np.asarray(inp["b_f"], f32)[0]

    def fm(vec, n):
        return np.ascontiguousarray(vec.reshape(n, 128).T)

    common = {}
    common["badaT"] = fm(b_ada, 6 * KC)
    common["n1g"] = fm(np.asarray(inp["norm1_g"], f32)[0], KC)
    common["n2g"] = fm(np.asarray(inp["norm2_g"], f32)[0], KC)
    common["qkn"] = np.ascontiguousarray(np.stack([np.asarray(inp[k], f32)[0] for k in
                                                   ("q_norm_a", "k_norm_a", "q_norm_b", "k_norm_b")], axis=1))
    common["relb"] = np.ascontiguousarray(rel_bias.reshape(1, -1))
    sk = np.stack([np.asarray(inp["sub_keys_1"], f32)[0], np.asarray(inp["sub_keys_2"], f32)[0]], axis=0)
    common["skT"] = np.ascontiguousarray(sk.transpose(2, 0, 1))
    common["ident"] = np.eye(128, dtype=f32)
    ii = np.arange(128)
    prevm = np.where(ii[:, None] >= ii[None, :], 0.0, NEG).astype(f32)
    diagm = np.where(ii[:, None] <= ii[None, :], 0.0, NEG).astype(f32)
    alln = np.full((128, 128), NEG, f32)
    common["maskn"] = np.ascontiguousarray(np.concatenate([prevm, diagm, prevm, diagm], axis=1))
    common["maskf"] = np.ascontiguousarray(np.concatenate([alln, diagm, prevm, diagm], axis=1))
    common["maskd"] = diagm

    def head_slab(colq, colk, colv):
        cols = np.concatenate([np.arange(colq, colq + 128), np.arange(colk, colk + 128), np.arange(colv, colv + 128)])
        w = w_in[:, cols]
        return w.reshape(KC, 128, 384).transpose(1, 0, 2)

    win_g = []
    for g in range(1):
        parts = []
        for i in range(nA):
            h = g * nA + i
            parts.append(head_slab(h * 128, WA + h * 128, 2 * WA + h * 128).ravel())
        for i in range(nB):
            h = g * nB + i
            parts.append(head_slab(3 * WA + h * 128, 3 * WA + WB + h * 128, 3 * WA + 2 * WB + h * 128).ravel())
        fzc = w_in[:, 3 * WA + 3 * WB + g * nB: 3 * WA + 3 * WB + (g + 1) * nB]
        parts.append(fzc.reshape(KC, 128, nB).transpose(1, 0, 2).ravel())
        win_g.append(np.concatenate(parts))
    wo_p = w_o
    NG = D // 256
    wo_l = wo_p.reshape(KC, 128, NG, 256).transpose(2, 1, 0, 3).ravel()
    wpq_l = w_pq.reshape(KC, 128, 16, 128).transpose(2, 1, 0, 3).ravel()
    uT_l = eu.reshape(128, 128, KC, 128).transpose(0, 3, 2, 1).ravel()
    ev_l = ev.ravel()

    def bias_tiles(h):
        out = np.zeros((3, 128, 256), f32)
        for bi, (w, d) in enumerate(cfg["BR"]):
            nw = w // d
            relp = ii[None, :] + 128 - ii[:, None]
            reld = ii[None, :] - ii[:, None]
            for o, rel in ((0, relp), (128, reld)):
                bk = _t5_bucket(np.clip(rel, 0, nw) * d)
                out[bi, :, o:o + 128] = rel_bias[bk, h]
        return out

    CH = cfg["CH"]

    def ishard(arr, tot, nr, rk):
        a = np.zeros(tot, f32)
        a[:arr.size] = arr
        return np.ascontiguousarray(a.reshape(-1, nr, CH)[:, rk, :].reshape(-1, 2048))

    cT_full = np.ascontiguousarray(c.reshape(4, KC, 128).transpose(2, 1, 0))
    wada_full = np.ascontiguousarray(w_ada.reshape(KC, 128, 6 * KC, 128).transpose(2, 1, 0, 3)).reshape(6 * KC, 128, KC * 128)
    wfull = {"win0": ishard(win_g[0], cfg["win_n"], 1, 0)}
    wfull["win1"] = wfull["win0"]
    bias_all = np.ascontiguousarray(np.stack([bias_tiles(i) for i in range(nA)], axis=0))
    for nm, arr, tot in (("wo", wo_l, cfg["wo_n"]), ("wpq", wpq_l, cfg["wpq_n"]),
                         ("uT", uT_l, cfg["uT_n"]), ("ev", ev_l, cfg["ev_n"])):
        wfull[nm] = ishard(arr, tot, 1, 0)
    maps = []
    for r in range(8):
        b, g = r // 2, r % 2
        m = dict(common)
        m["xf"] = np.ascontiguousarray(x[b])
        m["xh"] = np.ascontiguousarray(x[b, g * (S // 2):(g + 1) * (S // 2)])
        m["cT"] = cT_full
        m["wada"] = wada_full
        oh = np.zeros((128, 4), f32)
        oh[:, b] = 1.0
        m["oh"] = oh
        m["offs"] = np.array([[g * 128 * (S // 2), g * (S // 2) * D]], np.int32)
        m["win"] = wfull["win%d" % g]
        for nm in ("wo", "wpq", "uT", "ev"):
            m[nm] = wfull[nm]
        m["biasT"] = bias_all
        m["bf"] = np.ascontiguousarray(b_f.reshape(nB, 1))
        maps.append(m)
    return maps


def build(cfg, dbg=False, stop_after=None):
    D, S, KC, KC8 = cfg["D"], cfg["S"], cfg["KC"], cfg["KC8"]
    nA, nB = cfg["nA"], cfg["nB"]
    NL = nA + nB
    SH = S // 2
    NE = cfg["NE"]
    nc = bass.Bass("TRN2", target_bir_lowering=False)
    ctx = Ctx(nc)
    k = K(ctx)
    _cnt = [0]

    def uniq(name):
        _cnt[0] += 1
        return "s%d_%s" % (_cnt[0], name)

    def din(name, shape, dt=F32):
        return nc.dram_tensor(name, list(shape), dt, kind="ExternalInput")

    def dint(name, shape, dt):
        t = ctx.tile(nc.dram_tensor(name, list(shape), dt, kind="Internal"), name)
        t.dram = True
        return t

    xf_d = din("xf", [S, D])
    xh_d = din("xh", [SH, D])
    cT_d = din("cT", [128, KC, 4])
    wada_d = din("wada", [6 * KC, 128, KC * 128])
    badaT_d = din("badaT", [128, 6 * KC])
    n1g_d = din("n1g", [128, KC])
    n2g_d = din("n2g", [128, KC])
    qkn_d = din("qkn", [128, 4])
    relb_d = din("relb", [1, 32 * cfg["HA"]])
    skT_d = din("skT", [128, 2, 128])
    ident_d = din("ident", [128, 128])
    maskn_d = din("maskn", [128, 512])
    maskf_d = din("maskf", [128, 512])
    maskd_d = din("maskd", [128, 128])
    oh_d = din("oh", [128, 4])
    offs_d = din("offs", [1, 2], I32)
    biasT_d = din("biasT", [nA, 3, 128, 256])
    bf_d = din("bf", [nB, 1])
    wsh = {}
    for nm, tot, nr in (("win", cfg["win_n"], 1), ("wo", cfg["wo_n"], 1), ("wpq", cfg["wpq_n"], 1),
                        ("uT", cfg["uT_n"], 1), ("ev", cfg["ev_n"], 1)):
        n = tot // nr
        cc = 2048
        wsh[nm] = (din(nm, [n // cc, cc]), n // cc, cc, nr)
    out_d = nc.dram_tensor("out", [SH, D], F32, kind="ExternalOutput")
    dbg_out = {}

    def dbgout(name, shape, dt=F32):
        dbg_out[name] = nc.dram_tensor("dbg_" + name, list(shape), dt, kind="ExternalOutput")
        return dbg_out[name]

    wsh_b = {}
    wg = {}
    for nm, (h, n, cc, nr) in wsh.items():
        wg[nm] = dint(nm + "_g", [n, cc], BF16)
    gvec_d = dint("gvec", [2 * KC, 128], F32)
    KQ = KC // 4
    NPC = (SH // 256) * 4
    hTg_d = dint("hTg", [NPC * 2 * 128, KQ * 256], BF16)
    mixg_d = dint("mixg", [NL * 2 * 128, SH], BF16)
    x1_d = dint("x1", [SH, D], F32)
    h2T_d = dint("h2T", [128, KC * SH], BF16)

    G8 = [list(range(8))]
    GP = [[0, 1], [2, 3], [4, 5], [6, 7]]
    GQ = [[0, 2, 4, 6], [1, 3, 5, 7]]

    def cast_gather(nm, groups):
        h, n, cc, nr = wsh[nm]
        step = 2048
        for r0 in range(0, n, step):
            r1 = min(n, r0 + step)
            ctx.dma("gpsimd", wg[nm].v(wg[nm].h.ap()[r0:r1, :]), h.ap()[r0:r1, :])
        return lambda: None

    with ExitStack() as pes:
        def psb(name, shape, dt=F32):
            return ctx.tile(pes.enter_context(nc.sbuf_tensor(uniq(name), list(shape), dt)), name)

        ident = psb("ident", [128, 128])
        identb = psb("identb", [128, 128], BF16)
        ones_f = psb("ones_f", [128, 128])
        ones_b = psb("ones_b", [128, 128], BF16)
        msel = psb("msel", [128, 6 * KC])
        gm1 = psb("gm1", [128, KC])
        gm2 = psb("gm2", [128, KC])
        qkn = psb("qkn", [128, 4])
        qkg = psb("qkg", [128, 4])
        nbound = psb("nbound", [128, 2])
        offs_sb = psb("offs_sb", [1, 2], I32)
        reg_tok = pes.enter_context(nc.sync.register("r_tok"))
        reg_x = pes.enter_context(nc.sync.register("r_x"))

        def resnap(e):
            ctx.dyn["tok"] = e.snap(reg_tok)
            ctx.dyn["x"] = e.snap(reg_x)

        with ExitStack() as es:
            def sb(name, shape, dt=F32):
                return ctx.tile(es.enter_context(nc.sbuf_tensor(uniq(name), list(shape), dt)), name)

            pb = [ctx.tile(es.enter_context(nc.psum_tensor(uniq("pb%d" % i), [128, 512], F32)), "pb%d" % i) for i in range(8)]

            ctx.dma("sync", ident[:], ident_d.ap())
            ctx.dma("sync", offs_sb[:], offs_d.ap())
            ctx.dma("sync", qkn[:], qkn_d.ap())
            ctx.wait_all("sync")

            def ld(e):
                e.reg_load(reg_tok, offs_sb.h[:1, 0:1])
                e.reg_load(reg_x, offs_sb.h[:1, 1:2])
                resnap(e)
            ctx.raw("sync", ld)
            k.cp(identb[:], ident[:])
            k.memset(ones_f[:], 1.0)
            k.memset(ones_b[:], 1.0)

            g_win = cast_gather("win", GQ)
            g_wo = cast_gather("wo", G8)
            g_wpq = cast_gather("wpq", G8)

            cT = sb("cT", [128, KC, 4])
            sg = sb("sg", [128, KC, 4])
            ctx.dma("sync", cT[:], cT_d.ap())
            k.act(sg[:], cT[:], AF.Sigmoid)
            k.tt(cT[:], cT[:], sg[:], ALU.mult)
            wring = Ring([sb("wad%d" % i, [128, KC * 128]) for i in range(3)])
            ncc = 6 * KC
            for ccg in range(ncc):
                wt = wring.next()
                ctx.dma("sync", wt[:], wada_d.ap()[ccg])
                bank = pb[(ccg * 4) // 512]
                o = (ccg * 4) % 512
                for kc in range(KC):
                    k.mm(bank[:, o:o + 4], wt[:, kc * 128:(kc + 1) * 128], cT[:, kc, :], start=(kc == 0), stop=(kc == KC - 1))
            modp = sb("modp_s", [128, ncc * 4])
            for bi in range((ncc * 4 + 511) // 512):
                w = min(512, ncc * 4 - bi * 512)
                k.cp(modp[:, bi * 512: bi * 512 + w], pb[bi][:, 0:w])
            g_win()
            g_wo()
            g_wpq()
            g_uT = cast_gather("uT", G8)
            g_ev = cast_gather("ev", G8)
            modr = modp[:].re("p (c b) -> p c b", b=4)
            oh = sb("oh", [128, 4])
            ctx.dma("sync", oh[:], oh_d.ap())
            bada = sb("bada", [128, ncc])
            ctx.dma("sync", bada[:], badaT_d.ap())
            n1g = sb("n1g", [128, KC])
            n2g = sb("n2g", [128, KC])
            ctx.dma("sync", n1g[:], n1g_d.ap())
            ctx.dma("sync", n2g[:], n2g_d.ap())
            k.ts(msel[:], modr[:, :, 0], oh[:, 0:1], ALU.mult)
            for b in range(1, 4):
                k.stt(msel[:], modr[:, :, b], oh[:, b:b + 1], msel[:], ALU.mult, ALU.add)
            k.tt(msel[:], msel[:], bada[:], ALU.add)
            k.stt(gm1[:], msel[:, KC:2 * KC], 1.0, n1g[:], ALU.add, ALU.mult)
            k.stt(gm2[:], msel[:, 4 * KC:5 * KC], 1.0, n2g[:], ALU.add, ALU.mult)
            sh1 = msel[:, 0:KC]
            for i, o in enumerate((2 * KC, 5 * KC)):
                k.tr(pb[7][0:KC, i * 128:(i + 1) * 128], msel[:, o:o + KC], ident[:])
            gtmp = sb("gtmp", [KC, 256])
            k.cp(gtmp[:], pb[7][0:KC, 0:256])
            ctx.dma("sync", gvec_d.v(gvec_d.h.ap()[0:KC, :]), gtmp[:, 0:128])
            ctx.dma("sync", gvec_d.v(gvec_d.h.ap()[KC:2 * KC, :]), gtmp[:, 128:256])
            k.cp(qkg[:], qkn[:])
            k.ts(qkg[:, 0:1], qkn[:, 0:1], 128.0 ** -0.5, ALU.mult)
            k.ts(qkg[:, 2:3], qkn[:, 2:3], 128.0 ** -0.5, ALU.mult)
            gg = sb("gg", [128, 2])
            k.tt(gg[:, 0:1], qkn[:, 0:1], qkn[:, 1:2], ALU.mult)
            k.tt(gg[:, 1:2], qkn[:, 2:3], qkn[:, 3:4], ALU.mult)
            k.tr(pb[6][0:2, 0:128], gg[:], ident[:])
            ggT = sb("ggT", [2, 128])
            k.cp(ggT[:], pb[6][0:2, 0:128])
            bnd = sb("bnd", [2, 2])
            k.rmax_abs(bnd[:, 0:1], ggT[:])
            relb = sb("relb", [1, 32 * cfg["HA"]])
            ctx.dma("sync", relb[:], relb_d.ap())
            rbm = sb("rbm", [1, 1])
            k.rmax_abs(rbm[:], relb[:])
            k.ts(bnd[:, 0:1], bnd[:, 0:1], -(128.0 ** 0.5), ALU.mult)
            k.ts(rbm[:], rbm[:], -1.0, ALU.mult)
            k.tt(bnd[0:1, 0:1], bnd[0:1, 0:1], rbm[:], ALU.add)
            sel2 = sb("sel2", [2, 2])
            k.ts(sel2[:], ident[0:2, 0:2], bnd[:, 0:1], ALU.mult)
            k.mm(pb[6][:, 256:258], ones_f[0:2, :], sel2[:])
            k.cp(nbound[:], pb[6][:, 256:258])

            NT = S // 128
            xring = Ring([sb("xt%d" % i, [128, D]) for i in range(2)])
            sqj = sb("sqj", [128, D], BF16)
            hblk = Ring([sb("hblk%d" % i, [128, KC, 512], BF16) for i in range(2)])
            st = Ring([sb("st%d" % i, [128, 4]) for i in range(4)])
            pr = Ring(pb[0:6])
            hb = None
            for tt_ in range(NT):
                if tt_ % 4 == 0:
                    hb = hblk.next()
                xt = xring.next()
                ctx.dma("sync", xt[:], xf_d.ap()[tt_ * 128:(tt_ + 1) * 128, :])
                s_ = st.next()
                k.act(sqj[:], xt[:], AF.Square, accum=s_[:, 0:1])
                k.act(s_[:, 1:2], s_[:, 0:1], AF.Ln, bias=EPS, scale=1.0 / D)
                k.act(s_[:, 2:3], s_[:, 1:2], AF.Exp, scale=-0.5)
                k.ts(xt[:], xt[:], s_[:, 2:3], ALU.mult)
                for cg in range(KC // 4):
                    bank = pr.next()
                    for j in range(4):
                        c_ = cg * 4 + j
                        k.tr(bank[:, j * 128:(j + 1) * 128], xt[:, c_ * 128:(c_ + 1) * 128], ident[:])
                    for j in range(4):
                        c_ = cg * 4 + j
                        dst = hb[:, c_, (tt_ % 4) * 128:(tt_ % 4 + 1) * 128]
                        if j % 2 == 0:
                            k.ts(dst, bank[:, j * 128:(j + 1) * 128], gm1[:, c_:c_ + 1], ALU.mult, sh1[:, c_:c_ + 1], ALU.add)
                        else:
                            k.act(dst, bank[:, j * 128:(j + 1) * 128], AF.Identity, bias=sh1[:, c_:c_ + 1], scale=gm1[:, c_:c_ + 1])
                if tt_ % 4 == 3:
                    for sub in range(2):
                        tbg = (tt_ // 4) * 2 + sub
                        half, tbl = tbg // (SH // 256), tbg % (SH // 256)
                        for kq in range(4):
                            row = ((tbl * 4 + kq) * 2 + half) * 128
                            ctx.dma("sync", hTg_d.v(hTg_d.h.ap()[row:row + 128, :].rearrange("p (c t) -> p c t", t=256)),
                                    hb[:, kq * KQ:(kq + 1) * KQ, sub * 256:(sub + 1) * 256])
            if dbg:
                dm = dbgout("msel", [128, 6 * KC])
                ctx.dma("sync", dm.ap(), msel[:])
                dnb = dbgout("nbound", [128, 2])
                ctx.dma("sync", dnb.ap(), nbound[:])
                dh = dbgout("hTg", [NPC * 2 * 128, KQ * 256], BF16)
                ctx.dma("sync", dh.ap(), hTg_d.v(hTg_d.h.ap()))
            ctx.wait_all("sync")
            with nc.Block() as block:
                ctx.flush(block)

        if stop_after == 1:
            return nc, dbg_out

        NT = S // 128
        PBK = 256
        NPB = S // PBK

        def load_hblock(dst, tb):
            half, tbl = tb // (SH // 256), tb % (SH // 256)
            for kq in range(4):
                row = ((tbl * 4 + kq) * 2 + half) * 128
                ctx.dma("sync", dst[:, kq * KQ:(kq + 1) * KQ, :],
                        hTg_d.v(hTg_d.h.ap()[row:row + 128, :].rearrange("p (c t) -> p c t", t=256)))

        for phase in cfg.get("phases", ("A", "B")):
            with ExitStack() as es:
                def sb(name, shape, dt=F32):
                    return ctx.tile(es.enter_context(nc.sbuf_tensor(uniq(name), list(shape), dt)), name)

                pb = [ctx.tile(es.enter_context(nc.psum_tensor(uniq("pb%d" % i), [128, 512], F32)), "pb%d" % i) for i in range(8)]
                Wh = sb("Wh", [128, KC * 384], BF16)
                hring = Ring([sb("hTb%d" % i, [128, KC, PBK], BF16) for i in range(2)])
                QT = sb("QT", [128, S], BF16)
                KT = sb("KT", [128, S], BF16)
                VT = sb("VT", [128, S], BF16)
                sqr = Ring([sb("sqf%d" % i, [128, PBK]) for i in range(2)])
                rsr = Ring([sb("rs%d" % i, [128, PBK]) for i in range(2)])
                tmpr = Ring([sb("tmp%d" % i, [128, 512]) for i in range(3)])
                ptr = Ring([sb("PT%d" % i, [128, 512], BF16) for i in range(3)])
                mixT = sb("mixT", [128, S], BF16)
                sring = Ring([pb[4], pb[5], pb[0], pb[1]])
                numb, denb = pb[6], pb[7]
                misc = Ring([pb[2], pb[3]])
                nb_col = 0 if phase == "A" else 1
                gq = qkg[:, 0:1] if phase == "A" else qkg[:, 2:3]
                gk = qkg[:, 1:2] if phase == "A" else qkg[:, 3:4]
                nbnd = nbound[:, nb_col:nb_col + 1]

                if phase == "A":
                    Vd = [sb("Vd%d" % i, [128, NT, 128], BF16) for i in range(3)]
                    RNG = min(2048, S)
                    numA = sb("numA", [128, RNG])
                    denA = sb("denA", [128, RNG])
                    t1 = sb("t1", [128, RNG])
                    btr = Ring([sb("bt%d" % i, [128, 256]) for i in range(2)])
                    bnt = [sb("bn%d" % i, [128, 512]) for i in range(3)]
                    bft = [sb("bf%d" % i, [128, 512]) for i in range(3)]
                    maskn = sb("maskn", [128, 512])
                    maskf = sb("maskf", [128, 512])
                    ctx.dma("sync", maskn[:], maskn_d.ap())
                    ctx.dma("sync", maskf[:], maskf_d.ap())
                else:
                    Vtm = sb("Vtm", [128, NT, 128], BF16)
                    maskd = sb("maskd", [128, 128])
                    ctx.dma("sync", maskd[:], maskd_d.ap())
                    CS = sb("CS", [nB, S])
                    lg = sb("lg", [nB, S])
                    efr = Ring([sb("ef%d" % i, [nB, PBK]) for i in range(2)])
                    negF = sb("negF", [128, NT * nB])
                    fqr = Ring([sb("fqb%d" % i, [128, 512]) for i in range(2)])
                    sel_all = sb("sel_all", [nB, nB, 128])
                    Wfz = sb("Wfz", [128, KC * nB], BF16)
                    nbf = sb("nbf", [nB, 1])
                    ctx.dma("sync", Wfz[:], wg["win"].dap(NL * 128 * KC * 384, [[KC * nB, 128], [1, KC * nB]]))
                    ctx.dma("sync", nbf[:], bf_d.ap())
                    k.ts(nbf[:], nbf[:], -1.0, ALU.mult)
                    for h_ in range(nB):
                        k.cp(sel_all[:, h_, :], ident[0:nB, h_:h_ + 1].bc([nB, 128]))
                    for tb in range(NPB):
                        hb = hring.next()
                        load_hblock(hb, tb)
                        bank = misc.next()
                        for c_ in range(KC):
                            k.mm(bank[0:nB, 0:PBK], Wfz[:, c_ * nB:(c_ + 1) * nB], hb[:, c_, :], start=(c_ == 0), stop=(c_ == KC - 1))
                        ef = efr.next()
                        k.act(ef[:], bank[0:nB, 0:PBK], AF.Exp, bias=nbf[:, 0:1], scale=-1.0)
                        k.act(lg[:, tb * PBK:(tb + 1) * PBK], ef[:], AF.Ln, bias=1.0, scale=1.0)
                    ctx.op("vector", lambda e, o=CS.h[:], d=lg.h[:]:
                           e.tensor_tensor_scan(out=o, data0=d, data1=d, initial=0.0, op0=ALU.add, op1=ALU.bypass), [lg], [CS])
                    bank = misc.next()
                    for t_ in range(NT):
                        k.tr(bank[:, t_ * nB:(t_ + 1) * nB], CS[:, t_ * 128:(t_ + 1) * 128], ident[0:nB, 0:nB])
                    k.ts(negF[:], bank[:, 0:NT * nB], nbnd, ALU.add)
                    if dbg:
                        dcs = dbgout("CS", [nB, S])
                        ctx.dma("sync", dcs.ap(), CS[:])

                nh = nA if phase == "A" else nB
                for hl in range(nh):
                    slot = hl if phase == "A" else nA + hl
                    ctx.dma("sync", Wh[:], wg["win"].dap(slot * 128 * KC * 384, [[KC * 384, 128], [1, KC * 384]]))
                    for tb in range(NPB):
                        hb = hring.next()
                        load_hblock(hb, tb)
                        cols = slice(tb * PBK, (tb + 1) * PBK)
                        for j in range(3):
                            for c_ in range(KC):
                                k.mm(pb[j][:, 0:PBK], Wh[:, c_ * 384 + j * 128: c_ * 384 + (j + 1) * 128], hb[:, c_, :],
                                     start=(c_ == 0), stop=(c_ == KC - 1))
                        k.cp(VT[:, cols], pb[2][:, 0:PBK], eng="scalar")
                        for j, (dst, gn) in enumerate(((QT, gq), (KT, gk))):
                            sq = sqr.next()
                            rs = rsr.next()
                            k.act(sq[:], pb[j][:, 0:PBK], AF.Square)
                            k.mm(pb[3][:, j * PBK:(j + 1) * PBK], ones_f[:], sq[:])
                            k.act(rs[:], pb[3][:, j * PBK:(j + 1) * PBK], AF.Ln, bias=EPS, scale=1.0 / 128)
                            k.act(rs[:], rs[:], AF.Exp, scale=-0.5)
                            k.stt(dst[:, cols], pb[j][:, 0:PBK], gn, rs[:], ALU.mult, ALU.mult)

                    if cfg.get("lvl", 9) < 1:
                        continue
                    if phase == "A":
                        for br, (w_, d_) in enumerate(cfg["BR"]):
                            nblk = S // (128 * d_)
                            for b0 in range(0, NT, 4):
                                bank = misc.next()
                                bv = bank[:].bitcast(BF16)
                                for j in range(4):
                                    blk = b0 + j
                                    r_, n_ = blk // nblk, blk % nblk
                                    st_ = r_ + d_ * n_ * 128
                                    k.tr(bv[:, j * 128:(j + 1) * 128], VT[:, st_: st_ + d_ * 127 + 1: d_], identb[:])
                                k.cp(Vd[br][:, b0:b0 + 4, :].re("p a b -> p (a b)"), bv[:, 0:512], eng=("scalar" if (b0 // 4) % 2 else "vector"))
                            bt = btr.next()
                            ctx.dma("sync", bt[:], biasT_d.ap()[hl, br])
                            for hf in range(2):
                                k.tt(bnt[br][:, hf * 256:(hf + 1) * 256], bt[:], maskn[:, hf * 256:(hf + 1) * 256], ALU.add)
                                k.tt(bft[br][:, hf * 256:(hf + 1) * 256], bt[:], maskf[:, hf * 256:(hf + 1) * 256], ALU.add)
                        for rg in range(S // RNG if cfg.get("lvl", 9) >= 2 else 0):
                            for br, (w_, d_) in enumerate(cfg["BR"]):
                                nblk = S // (128 * d_)
                                per = RNG // (128 * d_)
                                for r_ in range(d_):
                                    n_lo, n_hi = rg * per, (rg + 1) * per
                                    for n0 in range(n_lo, n_hi, 2):
                                        nq = min(2, n_hi - n0)
                                        sp = sring.next()

                                        def tk_(n_):
                                            st_ = r_ + d_ * n_ * 128
                                            return slice(st_, st_ + d_ * 127 + 1, d_)
                                        for qi in range(nq):
                                            n_ = n0 + qi
                                            np_ = max(n_ - 1, 0)
                                            k.mm(sp[:, qi * 256: qi * 256 + 128], KT[:, tk_(np_)], QT[:, tk_(n_)])
                                            k.mm(sp[:, qi * 256 + 128: qi * 256 + 256], KT[:, tk_(n_)], QT[:, tk_(n_)])
                                        W_ = nq * 256
                                        tmp = tmpr.next()
                                        PT = ptr.next()
                                        bias_t = bft[br] if n0 == 0 else bnt[br]
                                        k.tt(tmp[:, 0:W_], sp[:, 0:W_], bias_t[:, 0:W_], ALU.add)
                                        k.act(PT[:, 0:W_], tmp[:, 0:W_], AF.Exp, bias=nbnd, scale=1.0)
                                        for qi in range(nq):
                                            n_ = n0 + qi
                                            np_ = max(n_ - 1, 0)
                                            o_ = slice(qi * 128, (qi + 1) * 128)
                                            k.mm(numb[:, o_], Vd[br][:, r_ * nblk + np_, :], PT[:, qi * 256: qi * 256 + 128], start=True, stop=False)
                                            k.mm(numb[:, o_], Vd[br][:, r_ * nblk + n_, :], PT[:, qi * 256 + 128: qi * 256 + 256], start=False, stop=True)
                                        for qi in range(nq):
                                            o_ = slice(qi * 128, (qi + 1) * 128)
                                            k.mm(denb[:, o_], ones_b[:], PT[:, qi * 256: qi * 256 + 128], start=True, stop=False)
                                            k.mm(denb[:, o_], ones_b[:], PT[:, qi * 256 + 128: qi * 256 + 256], start=False, stop=True)
                                        st_ = r_ + d_ * n0 * 128 - rg * RNG
                                        dsl = slice(st_, st_ + d_ * (nq * 128 - 1) + 1, d_)
                                        if br == 0:
                                            k.cp(numA[:, dsl], numb[:, 0:nq * 128], eng="scalar")
                                            k.cp(denA[:, dsl], denb[:, 0:nq * 128], eng="vector")
                                        else:
                                            k.tt(numA[:, dsl], numA[:, dsl], numb[:, 0:nq * 128], ALU.add)
                                            k.tt(denA[:, dsl], denA[:, dsl], denb[:, 0:nq * 128], ALU.add)
                            k.act(t1[:], denA[:], AF.Ln)
                            k.act(t1[:], t1[:], AF.Exp, scale=-1.0)
                            k.tt(mixT[:, rg * RNG:(rg + 1) * RNG], numA[:], t1[:], ALU.mult)
                    else:
                        for b0 in range(0, NT, 4):
                            bank = misc.next()
                            bv = bank[:].bitcast(BF16)
                            for j in range(4):
                                k.tr(bv[:, j * 128:(j + 1) * 128], VT[:, (b0 + j) * 128:(b0 + j + 1) * 128], identb[:])
                            k.cp(Vtm[:, b0:b0 + 4, :].re("p a b -> p (a b)"), bv[:, 0:512], eng=("scalar" if (b0 // 4) % 2 else "vector"))
                        QG = 512
                        for g_ in range(S // QG):
                            bank = misc.next()
                            k.mm(bank[:, 0:QG], sel_all[:, hl, :], CS[:, g_ * QG:(g_ + 1) * QG])
                            fqb = fqr.next()
                            k.cp(fqb[:], bank[:, 0:QG], eng="scalar")
                            nj = 4 * g_ + 4
                            sps = {}
                            sps[0] = sring.next()
                            k.mm(sps[0][:, 0:QG], KT[:, 0:128], QT[:, g_ * QG:(g_ + 1) * QG])
                            for j in range(nj):
                                if j + 1 < nj:
                                    sps[j + 1] = sring.next()
                                    c1 = max(0, j + 1 - 4 * g_) * 128
                                    k.mm(sps[j + 1][:, c1:QG], KT[:, (j + 1) * 128:(j + 2) * 128], QT[:, g_ * QG + c1:(g_ + 1) * QG])
                                sp = sps.pop(j)
                                m_ = j - 4 * g_
                                c0 = max(0, m_) * 128
                                tmp = tmpr.next()
                                PT = ptr.next()
                                k.tt(tmp[:, c0:QG], sp[:, c0:QG], fqb[:, c0:QG], ALU.subtract)
                                if m_ >= 0:
                                    k.tt(tmp[:, c0:c0 + 128], tmp[:, c0:c0 + 128], maskd[:], ALU.add)
                                k.act(PT[:, c0:QG], tmp[:, c0:QG], AF.Exp, bias=negF[:, j * nB + hl: j * nB + hl + 1], scale=1.0)
                                k.mm(numb[:, c0:QG], Vtm[:, j, :], PT[:, c0:QG], start=(j == 0), stop=(j == nj - 1))
                                k.mm(denb[:, c0:QG], ones_b[:], PT[:, c0:QG], start=(j == 0), stop=(j == nj - 1))
                            tl = tmpr.next()
                            k.act(tl[:, 0:QG], denb[:, 0:QG], AF.Ln)
                            k.act(tl[:, 0:QG], tl[:, 0:QG], AF.Exp, scale=-1.0)
                            k.tt(mixT[:, g_ * QG:(g_ + 1) * QG], numb[:, 0:QG], tl[:, 0:QG], ALU.mult)
                    for th in range(2):
                        pc = slot * 2 + th
                        ctx.dma("sync", mixg_d.v(mixg_d.h.ap()[pc * 128:(pc + 1) * 128, :]), mixT[:, th * SH:(th + 1) * SH])
                if phase == "B":
                    if dbg:
                        dmx = dbgout("mixl", [NL * 2 * 128, SH], BF16)
                        ctx.dma("sync", dmx.ap(), mixg_d.v(mixg_d.h.ap()))
                ctx.wait_all("sync")
                with nc.Block() as block:
                    ctx.raw("sync", resnap)
                    ctx.flush(block)
        if stop_after == 2:
            return nc, dbg_out

        def norm_tile(xt, xn, hb, col0, gm, sh, sqj, s_, prng):
            k.act(sqj[:], xt[:], AF.Square, accum=s_[:, 0:1])
            k.act(s_[:, 1:2], s_[:, 0:1], AF.Ln, bias=EPS, scale=1.0 / D)
            k.act(s_[:, 2:3], s_[:, 1:2], AF.Exp, scale=-0.5)
            k.ts(xn[:], xt[:], s_[:, 2:3], ALU.mult)
            for cg in range(KC // 4):
                bank = prng.next()
                for j in range(4):
                    c_ = cg * 4 + j
                    k.tr(bank[:, j * 128:(j + 1) * 128], xn[:, c_ * 128:(c_ + 1) * 128], ident[:])
                for j in range(4):
                    c_ = cg * 4 + j
                    dst = hb[:, c_, col0:col0 + 128]
                    if j % 2 == 0:
                        k.ts(dst, bank[:, j * 128:(j + 1) * 128], gm[:, c_:c_ + 1], ALU.mult, sh[:, c_:c_ + 1], ALU.add)
                    else:
                        k.act(dst, bank[:, j * 128:(j + 1) * 128], AF.Identity, bias=sh[:, c_:c_ + 1], scale=gm[:, c_:c_ + 1])

        with ExitStack() as es:
            def sb(name, shape, dt=F32):
                return ctx.tile(es.enter_context(nc.sbuf_tensor(uniq(name), list(shape), dt)), name)

            pb = [ctx.tile(es.enter_context(nc.psum_tensor(uniq("pb%d" % i), [128, 512], F32)), "pb%d" % i) for i in range(8)]
            g1b = sb("g1b", [128, D])
            ctx.dma("sync", g1b[:], gvec_d.dap(0, [[0, 128], [1, D]]))
            sh2 = msel[:, 3 * KC:4 * KC]
            mring = Ring([sb("mixb%d" % i, [128, KC, 256], BF16) for i in range(2)])
            wring = Ring([sb("wo%d" % i, [128, KC * 256], BF16) for i in range(2)])
            ytl = [sb("y%d" % i, [128, D]) for i in range(2)]
            xring = Ring([sb("xr%d" % i, [128, D]) for i in range(2)])
            sqj = sb("sqj", [128, D], BF16)
            h2b = Ring([sb("h2b%d" % i, [128, KC, 256], BF16) for i in range(2)])
            st = Ring([sb("st%d" % i, [128, 4]) for i in range(4)])
            pyr = Ring(pb[0:4])
            ptr_ = Ring(pb[4:8])
            mdims = [[SH, 128], [2 * 128 * SH, KC], [1, 256]]
            for tb in range(SH // 256):
                mb = mring.next()
                ctx.dma("sync", mb[:], mixg_d.dap(0, mdims),
                        in_fn=lambda tb=tb: bass.AP(mixg_d.h, ctx.dyn["tok"] + tb * 256, [list(d) for d in mdims]))
                for ng in range(D // 256):
                    wt = wring.next()
                    ctx.dma("sync", wt[:], wg["wo"].dap(ng * 128 * KC * 256, [[KC * 256, 128], [1, KC * 256]]))
                    for tt in range(2):
                        bank = pyr.next()
                        for c_ in range(KC):
                            k.mm(bank[:, 0:256], mb[:, c_, tt * 128:(tt + 1) * 128], wt[:, c_ * 256:(c_ + 1) * 256],
                                 start=(c_ == 0), stop=(c_ == KC - 1))
                        k.tt(ytl[tt][:, ng * 256:(ng + 1) * 256], bank[:, 0:256], g1b[:, ng * 256:(ng + 1) * 256], ALU.mult)
                hb = h2b.next()
                for tt in range(2):
                    ti = tb * 2 + tt
                    xr = xring.next()
                    ctx.dma("sync", xr[:], xh_d.ap()[ti * 128:(ti + 1) * 128, :])
                    y = ytl[tt]
                    k.tt(y[:], y[:], xr[:], ALU.add)
                    ctx.dma("sync", x1_d.v(x1_d.h.ap()[ti * 128:(ti + 1) * 128, :]), y[:])
                    norm_tile(y, xr, hb, tt * 128, gm2, sh2, sqj, st.next(), ptr_)
                ctx.dma("sync", h2T_d.dap(tb * 256, [[KC * SH, 128], [SH, KC], [1, 256]]), hb[:])
            if dbg:
                d1 = dbgout("x1", [SH, D])
                ctx.dma("sync", d1.ap(), x1_d.v(x1_d.h.ap()))
                d2 = dbgout("h2T", [128, KC * SH], BF16)
                ctx.dma("sync", d2.ap(), h2T_d.v(h2T_d.h.ap()))
            ctx.wait_all("sync")
            with nc.Block() as block:
                ctx.raw("sync", resnap)
                ctx.ops["sync"].insert(0, ctx.ops["sync"].pop())
                ctx.flush(block)
        if stop_after == 3:
            return nc, dbg_out

        with ExitStack() as es:
            def sb(name, shape, dt=F32):
                return ctx.tile(es.enter_context(nc.sbuf_tensor(uniq(name), list(shape), dt)), name)

            pbh = [es.enter_context(nc.psum_tensor(uniq("pb%d" % i), [128, 512], F32)) for i in range(8)]
            pb = [ctx.tile(h_, "pb%d" % i) for i, h_ in enumerate(pbh)]
            gring = Ring([pb[6], pb[7]])
            g2 = msel[:, 5 * KC:6 * KC]
            skf = sb("skf", [128, 2, 128])
            skb = sb("skb", [128, 2, 128], BF16)
            ctx.dma("sync", skf[:], skT_d.ap())
            k.cp(skb[:], skf[:])
            h2r = Ring([sb("h2r%d" % i, [128, KC, 256], BF16) for i in range(1)])
            wqr = Ring([sb("wq%d" % i, [128, KC * 128], BF16) for i in range(2)])
            qT = sb("qT", [128, 16, 256], BF16)
            S12 = [sb("S12_%d" % i, [128, 16, 128]) for i in range(2)]
            thr = [sb("thr%d" % i, [128, 8]) for i in range(2)]
            b3 = [sb("b3_%d" % i, [128, 8]) for i in range(2)]
            v1r = Ring([sb("v1_%d" % i, [128, 16]) for i in range(2)])
            v2r = Ring([sb("v2_%d" % i, [128, 16]) for i in range(2)])
            vsr = Ring([sb("vs_%d" % i, [128, 16]) for i in range(2)])
            smr = Ring([sb("sm_%d" % i, [128, 128]) for i in range(2)])
            cdr = Ring([sb("cd_%d" % i, [128, 256]) for i in range(2)])
            cmr = Ring([sb("cm_%d" % i, [128, 256]) for i in range(2)])
            zr = Ring([sb("z_%d" % i, [128, 4]) for i in range(2)])
            j16 = sb("j16", [128, 16])
            tmpa = Ring([sb("tmpa%d" % i, [128, 1024]) for i in range(2)])
            tmpe = Ring([sb("tmpe%d" % i, [128, 1024]) for i in range(2)])
            tmp2 = Ring([sb("tmp2%d" % i, [128, 1024], BF16) for i in range(2)])
            Ggr = [Ring([sb("Gg%d_%d" % (t_, i), [128, 2048], BF16) for i in range(2)]) for t_ in range(2)]
            Ur = Ring([sb("U%d" % i, [128, KC * 128], BF16) for i in range(4)])
            gel = Ring([sb("gel%d" % i, [128, 256]) for i in range(3)])
            aTr = Ring([sb("aT%d" % i, [128, 16, 256], BF16) for i in range(2)])
            Vr = Ring([sb("Vs%d" % i, [128, 512], BF16) for i in range(12)])
            yacc = sb("yacc", [128, KC, 256])
            x1r = Ring([sb("x1p%d" % i, [128, 512]) for i in range(3)])
            aring = Ring([pb[0], pb[1]])
            accb = pb[2:6]
            NEG_ = NEG
            for tb in range(SH // 256):
                h2 = h2r.next()
                ctx.dma("sync", h2[:], h2T_d.dap(tb * 256, [[KC * SH, 128], [SH, KC], [1, 256]]))
                for jc in range(16):
                    wq = wqr.next()
                    ctx.dma("sync", wq[:], wg["wpq"].dap(jc * 128 * KC * 128, [[KC * 128, 128], [1, KC * 128]]))
                    bank = aring.next()
                    for c_ in range(KC):
                        k.mm(bank[:, 0:256], wq[:, c_ * 128:(c_ + 1) * 128], h2[:, c_, :], start=(c_ == 0), stop=(c_ == KC - 1))
                    k.cp(qT[:, jc, :], bank[:, 0:256], eng=("scalar" if jc % 2 else "vector"))
                for tt in range(2):
                    for q4 in range(4):
                        bank = aring.next()
                        for j in range(4):
                            jc = q4 * 4 + j
                            k.mm(bank[:, j * 128:(j + 1) * 128], qT[:, jc, tt * 128:(tt + 1) * 128], skb[:, jc % 2, :])
                        k.cp(S12[tt][:, q4 * 4:(q4 + 1) * 4, :].re("p a b -> p (a b)"), bank[:, 0:512], eng=("scalar" if q4 % 2 else "vector"))
                    for h_ in range(8):
                        s1 = S12[tt][:, 2 * h_, :]
                        s2 = S12[tt][:, 2 * h_ + 1, :]
                        v1, v2, vs = v1r.next(), v2r.next(), vsr.next()
                        for (sv, vv) in ((s1, v1), (s2, v2)):
                            sm = smr.next()
                            k.max8(vv[:, 0:8], sv)
                            k.mrep(sm[:], vv[:, 0:8], sv)
                            k.max8(vv[:, 8:16], sm[:])
                        cd, cm = cdr.next(), cmr.next()
                        k.tt(cd[:].re("p (a b) -> p a b", b=16), v1[:].unsq(2).bc([128, 16, 16]), v2[:].unsq(1).bc([128, 16, 16]), ALU.add)
                        k.max8(vs[:, 0:8], cd[:])
                        k.mrep(cm[:], vs[:, 0:8], cd[:])
                        k.max8(vs[:, 8:16], cm[:])
                        z = zr.next()
                        k.ts(z[:, 0:1], vs[:, 0:1], -1.0, ALU.mult)
                        k.act(j16[:], vs[:], AF.Exp, bias=z[:, 0:1], scale=1.0, accum=z[:, 1:2])
                        k.act(z[:, 2:3], z[:, 1:2], AF.Ln)
                        k.tt(b3[tt][:, h_:h_ + 1], z[:, 0:1], z[:, 2:3], ALU.subtract)
                        k.cp(thr[tt][:, h_:h_ + 1], vs[:, 15:16])
                for eg in range(NE // 2048):
                    Gg = [Ggr[0].next(), Ggr[1].next()]
                    for tt in range(2):
                        for h_ in range(8):
                            s1 = S12[tt][:, 2 * h_, :]
                            s2 = S12[tt][:, 2 * h_ + 1, :]
                            for sub in range(2):
                                e0 = eg * 16 + sub * 8
                                ta, te = tmpa.next(), tmpe.next()
                                k.tt(ta[:].re("p (a b) -> p a b", b=128), s1[:, e0:e0 + 8].unsq(2).bc([128, 8, 128]),
                                     s2.unsq(1).bc([128, 8, 128]), ALU.add)
                                k.act(te[:], ta[:], AF.Exp, bias=b3[tt][:, h_:h_ + 1], scale=1.0)
                                gdst = Gg[tt][:, sub * 1024:(sub + 1) * 1024]
                                if h_ == 0:
                                    k.stt(gdst, ta[:], thr[tt][:, h_:h_ + 1], te[:], ALU.is_ge, ALU.mult)
                                else:
                                    t2 = tmp2.next()
                                    k.stt(t2[:], ta[:], thr[tt][:, h_:h_ + 1], te[:], ALU.is_ge, ALU.mult)
                                    k.tt(gdst, gdst, t2[:], ALU.add, eng="gpsimd")
                    aT = aTr.next()
                    for ec in range(16):
                        U = Ur.next()
                        ctx.dma("sync", U[:], wg["uT"].dap((eg * 16 + ec) * 128 * KC * 128, [[KC * 128, 128], [1, KC * 128]]))
                        gs = gring.next()
                        gv = gs[:].bitcast(BF16)[:, 0:256]
                        for tt in range(2):
                            k.tr(gv[:, tt * 128:(tt + 1) * 128], Gg[tt][:, ec * 128:(ec + 1) * 128], identb[:])
                        bank = aring.next()
                        for c_ in range(KC):
                            k.mm(bank[:, 0:256], U[:, c_ * 128:(c_ + 1) * 128], h2[:, c_, :], start=(c_ == 0), stop=(c_ == KC - 1))
                        ge = gel.next()
                        k.act(ge[:], bank[:, 0:256], AF.Gelu)
                        k.tt(aT[:, ec, :], ge[:], gv, ALU.mult)
                    for dg in range(KC // 4):
                        for ec in range(16):
                            Vt = Vr.next()
                            ctx.dma("sync", Vt[:], wg["ev"].dap(((eg * 16 + ec) * 128) * D + dg * 512, [[D, 128], [1, 512]]))
                            for i in range(4):
                                k.mm(accb[i][:, 0:256], Vt[:, i * 128:(i + 1) * 128], aT[:, ec, :], start=(ec == 0), stop=(ec == 15))
                        for i in range(4):
                            dc = dg * 4 + i
                            if eg == 0:
                                k.ts(yacc[:, dc, :], accb[i][:, 0:256], g2[:, dc:dc + 1], ALU.mult)
                            else:
                                k.stt(yacc[:, dc, :], accb[i][:, 0:256], g2[:, dc:dc + 1], yacc[:, dc, :], ALU.mult, ALU.add)
                for tt in range(2):
                    ti = tb * 2 + tt
                    for cg in range(KC // 4):
                        bank = aring.next()
                        for j in range(4):
                            k.tr(bank[:, j * 128:(j + 1) * 128], yacc[:, cg * 4 + j, tt * 128:(tt + 1) * 128], ident[:])
                        xp = x1r.next()
                        ctx.dma("sync", xp[:], x1_d.v(x1_d.h.ap()[ti * 128:(ti + 1) * 128, cg * 512:(cg + 1) * 512]))
                        k.tt(xp[:], xp[:], bank[:, 0:512], ALU.add)
                        ctx.dma("sync", out_d.ap()[ti * 128:(ti + 1) * 128, cg * 512:(cg + 1) * 512], xp[:])
            ctx.wait_all("sync")
            with nc.Block() as block:
                ctx.flush(block)
    return nc, dbg_out


_CACHE = {}


def kernel(**inputs):
    D = int(np.asarray(inputs["x"]).shape[2])
    S = int(np.asarray(inputs["x"]).shape[1])
    cfg = make_cfg(D, S)
    maps = host_prep(inputs, cfg)
    key = (D, S)
    if key not in _CACHE:
        _CACHE[key] = build(cfg)[0]
    nc = _CACHE[key]
    res = run_bass_kernel_spmd(nc, maps, core_ids=list(range(8))).results
    SH = S // 2
    out = np.empty((4, S, D), np.float32)
    for r in range(8):
        b, g = r // 2, r % 2
        out[b, g * SH:(g + 1) * SH] = np.asarray(res[r]["out"], np.float32)
    return out
```
